# Optimizing a Trainium2 kernel written in Bass

```python
import math
import jax
import jax.numpy as jnp
from jax import lax
import numpy as np

D_MODEL = 1024
BATCH = 4
SEQ = 8192
DEPTH = 2

GRID_W = 64
CTX_LEN = 256

CONV_W = 4
CONV_PAD_L = 2
CONV_PAD_R = 1
CHUNK = 64

RG_WIDTH = D_MODEL
RG_HEADS = 8
RG_BLOCK = RG_WIDTH // RG_HEADS
RG_C = 8.0

SSD_INNER = D_MODEL
SSD_HEADDIM = 64
SSD_HEADS = SSD_INNER // SSD_HEADDIM
SSD_STATE = 128
SSD_GROUPS = 2
SSD_HPG = SSD_HEADS // SSD_GROUPS
SSD_CONV_DIM = SSD_INNER + 2 * SSD_GROUPS * SSD_STATE

GDN_HEADS = 8
GDN_DK = 128
GDN_DV = 128
GDN_QK = GDN_HEADS * GDN_DK
GDN_V = GDN_HEADS * GDN_DV

HG_HEADS = 8
HG_DK = 128
HG_DV = 128
HG_K = HG_HEADS * HG_DK
HG_V = HG_HEADS * HG_DV

MIX_WIDTH = RG_WIDTH + SSD_INNER
AB_SIZES = (RG_WIDTH, RG_WIDTH, SSD_INNER, SSD_CONV_DIM, 2 * SSD_HEADS)
CD_SIZES = (2 * GDN_QK + GDN_V, GDN_V, 2 * GDN_HEADS, 2 * GDN_HEADS, HG_K, 2 * HG_K, HG_V, HG_V)
AB_IN = sum(AB_SIZES)
CD_IN = sum(CD_SIZES)

N_GROUPS = 4
EXPERTS_PER_GROUP = 8
N_EXPERTS = N_GROUPS * EXPERTS_PER_GROUP
TOP_K = 2
D_EXPERT = 512
MOE_BLOCK = 256

N_EVEN = (DEPTH + 1) // 2
N_ODD = DEPTH // 2
ALPHA = (2 * DEPTH) ** 0.25
BETA = (8 * DEPTH) ** -0.25
LN_EPS = 1e-5
RMS_EPS = 1e-6
F32 = jnp.float32

kernel_name = 'hybrid_rglru_ssd_gdn_hgrn2_hmoe_dit'


def split_cols(z, sizes):
    return jnp.split(z, [int(s) for s in np.cumsum(sizes)[:-1]], axis=-1)


def layer_norm(x, g, b):
    xf = x.astype(F32)
    xc = xf - jnp.mean(xf, -1, keepdims=True)
    var = jnp.mean(xc * xc, -1, keepdims=True)
    return (xc * lax.rsqrt(var + LN_EPS) * g + b).astype(x.dtype)


def l2norm(t):
    return t * lax.rsqrt(jnp.sum(t * t, -1, keepdims=True) + RMS_EPS)


def modulate(x, shift, scale):
    return x * (1 + scale) + shift


def short_conv(x, w, b, rows):
    if rows is not None:
        bsz, n, ch = x.shape
        return short_conv(x.reshape(bsz * rows, GRID_W, ch), w, b, None).reshape(bsz, n, ch)
    n = x.shape[1]
    xp = jnp.pad(x, ((0, 0), (CONV_PAD_L, CONV_PAD_R), (0, 0)))
    y = sum(xp[:, j:j + n] * w[j] for j in range(CONV_W))
    return y if b is None else y + b


def block_mask(strict=False):
    return jnp.tril(jnp.ones((CHUNK, CHUNK), bool), -1 if strict else 0)


def chunked_scan(step, state0, xs):
    def to_chunks(t):
        b, n = t.shape[:2]
        return jnp.swapaxes(t.astype(F32).reshape(b, n // CHUNK, CHUNK, *t.shape[2:]), 0, 1)
    state, ys = lax.scan(step, state0, tuple(to_chunks(t) for t in xs))
    ys = jnp.swapaxes(ys, 0, 1)
    return ys.reshape(ys.shape[0], -1, *ys.shape[3:]), state


def bidir(scan_fn, ctx_dirs, lat_dirs):
    flip = lambda ts: tuple(jnp.flip(t, axis=1) for t in ts)
    y_ctx_f, s_f = scan_fn(*ctx_dirs[0], None)
    y_lat_f, _ = scan_fn(*lat_dirs[0], s_f)
    y_ctx_b, s_b = scan_fn(*flip(ctx_dirs[1]), None)
    y_lat_b, _ = scan_fn(*flip(lat_dirs[1]), s_b)
    return y_lat_f + jnp.flip(y_lat_b, 1), y_ctx_f + jnp.flip(y_ctx_b, 1)


def rglru_gates(xc, wa, ba, wx, bx, lam):
    bsz, n, ch = xc.shape
    xh = xc.reshape(bsz, n, RG_HEADS, RG_BLOCK)
    r = jax.nn.sigmoid(jnp.einsum('bthi,hij->bthj', xh, wa).reshape(bsz, n, ch) + ba)
    i = jax.nn.sigmoid(jnp.einsum('bthi,hij->bthj', xh, wx).reshape(bsz, n, ch) + bx)
    log_a = -RG_C * r * jax.nn.softplus(-lam)
    u = jnp.sqrt(-jnp.expm1(2.0 * log_a)) * (i * xc)
    return (u.astype(F32), log_a.astype(F32))


def rglru_scan(u, log_a, h0):
    def combine(left, right):
        a_l, b_l = left
        a_r, b_r = right
        return a_l * a_r, a_r * b_l + b_r
    a_cum, h = lax.associative_scan(combine, (jnp.exp(log_a), u), axis=1)
    if h0 is not None:
        h = h + a_cum * h0[:, None, :]
    return h, h[:, -1]


def ssd_scan(xdt, log_a, b_in, c_in, s0):
    bsz, _, g, r, p = xdt.shape
    nst = b_in.shape[-1]
    if s0 is None:
        s0 = jnp.zeros((bsz, g, r, nst, p), F32)
    mask = block_mask()

    def step(S, inp):
        xq, aq, bq, cq = inp
        cum = jnp.cumsum(aq, axis=1)
        dec = jnp.exp(jnp.where(mask[None, :, :, None, None], cum[:, :, None] - cum[:, None], -jnp.inf))
        scores = jnp.einsum('btgn,bsgn->btsg', cq, bq)
        y = jnp.einsum('btsgr,bsgrp->btgrp', scores[..., None] * dec, xq)
        y = y + jnp.einsum('btgn,bgrnp->btgrp', cq, S) * jnp.exp(cum)[..., None]
        w_end = jnp.exp(cum[:, -1:] - cum)
        S = S * jnp.exp(cum[:, -1])[..., None, None] + jnp.einsum('bsgn,bsgr,bsgrp->bgrnp', bq, w_end, xq)
        return S, y
    return chunked_scan(step, s0, (xdt, log_a, b_in, c_in))


def ab_features(z, rows, rg_conv_w, rg_conv_b, rg_gate_a_w, rg_gate_a_b, rg_gate_x_w, rg_gate_x_b, rg_lambda,
                ssd_conv_w, ssd_conv_b, ssd_a_log, ssd_dt_bias):
    bsz, n, _ = z.shape
    rg_x, rg_y, ssd_z, ssd_xbc, ssd_dt = split_cols(z, AB_SIZES)
    xc = short_conv(rg_x, rg_conv_w, rg_conv_b, rows)
    rg_dirs = tuple(rglru_gates(xc, rg_gate_a_w[d], rg_gate_a_b[d], rg_gate_x_w[d], rg_gate_x_b[d], rg_lambda[d])
                    for d in range(2))
    xbc = jax.nn.silu(short_conv(ssd_xbc, ssd_conv_w, ssd_conv_b, rows))
    xs, b_in, c_in = split_cols(xbc, (SSD_INNER, SSD_GROUPS * SSD_STATE, SSD_GROUPS * SSD_STATE))
    xs = xs.reshape(bsz, n, SSD_GROUPS, SSD_HPG, SSD_HEADDIM)
    b_in = b_in.reshape(bsz, n, SSD_GROUPS, SSD_STATE)
    c_in = c_in.reshape(bsz, n, SSD_GROUPS, SSD_STATE)
    dt_raw = ssd_dt.reshape(bsz, n, 2, SSD_GROUPS, SSD_HPG)
    ssd_dirs = []
    for d in range(2):
        dt = jax.nn.softplus(dt_raw[:, :, d] + ssd_dt_bias[d].reshape(SSD_GROUPS, SSD_HPG))
        log_a = -jnp.exp(ssd_a_log[d]).reshape(SSD_GROUPS, SSD_HPG) * dt
        ssd_dirs.append((xs * dt[..., None], log_a, b_in, c_in))
    return rg_dirs, tuple(ssd_dirs), (rg_y, ssd_z, xs)


def ab_output(h_rg, y_ssd, rg_y, ssd_z, xs, ssd_d, ssd_norm_g, out_w):
    bsz, n = rg_y.shape[:2]
    a_out = h_rg * jax.nn.gelu(rg_y)
    y = (y_ssd + ssd_d.reshape(SSD_GROUPS, SSD_HPG, 1) * xs).reshape(bsz, n, SSD_GROUPS, -1)
    y = y * jax.nn.silu(ssd_z).reshape(bsz, n, SSD_GROUPS, -1)
    y = (y * lax.rsqrt(jnp.mean(y * y, -1, keepdims=True) + RMS_EPS)).reshape(bsz, n, SSD_INNER) * ssd_norm_g
    return jnp.concatenate([a_out, y], -1).astype(out_w.dtype) @ out_w


def mixer_ab(u_lat, u_ctx, rows, in_w, out_w, rg_conv_w, rg_conv_b, rg_gate_a_w, rg_gate_a_b, rg_gate_x_w,
             rg_gate_x_b, rg_lambda, ssd_conv_w, ssd_conv_b, ssd_a_log, ssd_dt_bias, ssd_d, ssd_norm_g, with_ctx_out):
    def feats(u, r):
        return ab_features((u @ in_w).astype(F32), r, rg_conv_w, rg_conv_b, rg_gate_a_w, rg_gate_a_b, rg_gate_x_w,
                           rg_gate_x_b, rg_lambda, ssd_conv_w, ssd_conv_b, ssd_a_log, ssd_dt_bias)
    lat, ctx = feats(u_lat, rows), feats(u_ctx, None)
    h_rg_lat, h_rg_ctx = bidir(rglru_scan, ctx[0], lat[0])
    y_ssd_lat, y_ssd_ctx = bidir(ssd_scan, ctx[1], lat[1])
    y_lat = ab_output(h_rg_lat, y_ssd_lat, *lat[2], ssd_d, ssd_norm_g, out_w)
    y_ctx = ab_output(h_rg_ctx, y_ssd_ctx, *ctx[2], ssd_d, ssd_norm_g, out_w) if with_ctx_out else None
    return y_lat, y_ctx


def gdn_scan(q, k, v, g, beta, s0):
    bsz, _, h, dk = q.shape
    dv = v.shape[-1]
    if s0 is None:
        s0 = jnp.zeros((bsz, h, dk, dv), F32)
    incl, strict = block_mask(), block_mask(strict=True)
    eye = jnp.eye(CHUNK, dtype=F32)

    def step(S, inp):
        qq, kk, vv, gg, bb = inp
        cum = jnp.cumsum(gg, axis=1)
        cum_h = jnp.swapaxes(cum, 1, 2)
        L = jnp.exp(jnp.where(incl, cum_h[..., :, None] - cum_h[..., None, :], -jnp.inf))
        kb = kk * bb[..., None]
        m = jnp.where(strict, jnp.einsum('bthk,bshk->bhts', kb, kk) * L, 0.0)
        t_mat = lax.linalg.triangular_solve(eye + m, jnp.broadcast_to(eye, m.shape), left_side=True, lower=True,
                                            unit_diagonal=True)
        u = jnp.einsum('bhts,bshv->bhtv', t_mat, vv * bb[..., None])
        w = jnp.einsum('bhts,bshk->bhtk', t_mat, kb * jnp.exp(cum)[..., None])
        v_new = u - jnp.einsum('bhtk,bhkv->bhtv', w, S)
        attn = jnp.einsum('bthk,bshk->bhts', qq, kk) * L
        o = jnp.einsum('bthk,bhkv->bhtv', qq * jnp.exp(cum)[..., None], S) + jnp.einsum('bhts,bhsv->bhtv', attn, v_new)
        last = cum[:, -1]
        S = S * jnp.exp(last)[..., None, None] + jnp.einsum(
            'bshk,bhsv->bhkv', kk * jnp.exp(last[:, None] - cum)[..., None], v_new)
        return S, jnp.swapaxes(o, 1, 2)
    return chunked_scan(step, s0, (q, k, v, g, beta))


def hgrn_scan(q, k, v, log_f, s0):
    bsz, _, h, dk = q.shape
    dv = v.shape[-1]
    if s0 is None:
        s0 = jnp.zeros((bsz, h, dk, dv), F32)
    mask = block_mask()

    def step(S, inp):
        qq, kk, vv, lf = inp
        cum = jnp.cumsum(lf, axis=1)
        dec = jnp.exp(jnp.where(mask[None, :, :, None, None], cum[:, :, None] - cum[:, None], -jnp.inf))
        attn = jnp.einsum('bthk,btshk,bshk->bhts', qq, dec, kk)
        o = jnp.einsum('bhts,bshv->bthv', attn, vv) + jnp.einsum('bthk,bhkv->bthv', qq * jnp.exp(cum), S)
        last = cum[:, -1]
        S = S * jnp.exp(last)[..., None] + jnp.einsum('bshk,bshv->bhkv', kk * jnp.exp(last[:, None] - cum), vv)
        return S, o
    return chunked_scan(step, s0, (q, k, v, log_f))


def cd_features(z, rows, gdn_conv_w, gdn_a_log, gdn_dt_bias, lb):
    bsz, n, _ = z.shape
    qkv, g_gdn, beta_raw, a_raw, hq, hf, hi, g_hg = split_cols(z, CD_SIZES)
    qkv = jax.nn.silu(short_conv(qkv, gdn_conv_w, None, rows))
    q, k, v = split_cols(qkv, (GDN_QK, GDN_QK, GDN_V))
    q = l2norm(q.reshape(bsz, n, GDN_HEADS, GDN_DK)) * GDN_DK ** -0.5
    k = l2norm(k.reshape(bsz, n, GDN_HEADS, GDN_DK))
    v = v.reshape(bsz, n, GDN_HEADS, GDN_DV)
    beta = jax.nn.sigmoid(beta_raw.reshape(bsz, n, 2, GDN_HEADS))
    a = a_raw.reshape(bsz, n, 2, GDN_HEADS)
    gdn_dirs = tuple((q, k, v, -jnp.exp(gdn_a_log[d]) * jax.nn.softplus(a[:, :, d] + gdn_dt_bias[d]), beta[:, :, d])
                     for d in range(2))
    hq = hq.reshape(bsz, n, HG_HEADS, HG_DK)
    hi = hi.reshape(bsz, n, HG_HEADS, HG_DV)
    lbh = lb.reshape(HG_HEADS, HG_DK)
    f = lbh + (1 - lbh) * jax.nn.sigmoid(hf.reshape(bsz, n, 2, HG_HEADS, HG_DK))
    hg_dirs = tuple((hq, 1 - f[:, :, d], hi, jnp.log(f[:, :, d])) for d in range(2))
    return gdn_dirs, hg_dirs, (g_gdn, g_hg)


def gated_head_norm(o, gate, g):
    o = o * lax.rsqrt(jnp.mean(o * o, -1, keepdims=True) + RMS_EPS) * g
    return o.reshape(gate.shape) * jax.nn.silu(gate)


def mixer_cd(u_lat, u_ctx, rows, in_w, out_w, gdn_conv_w, gdn_a_log, gdn_dt_bias, gdn_norm_g, hg_norm_g, lb,
             with_ctx_out):
    def feats(u, r):
        return cd_features((u @ in_w).astype(F32), r, gdn_conv_w, gdn_a_log, gdn_dt_bias, lb)

    def out(o_gdn, o_hg, gates):
        y = jnp.concatenate([gated_head_norm(o_gdn, gates[0], gdn_norm_g), gated_head_norm(o_hg, gates[1], hg_norm_g)], -1)
        return y.astype(out_w.dtype) @ out_w
    lat, ctx = feats(u_lat, rows), feats(u_ctx, None)
    o_gdn_lat, o_gdn_ctx = bidir(gdn_scan, ctx[0], lat[0])
    o_hg_lat, o_hg_ctx = bidir(hgrn_scan, ctx[1], lat[1])
    y_lat = out(o_gdn_lat, o_hg_lat, lat[2])
    y_ctx = out(o_gdn_ctx, o_hg_ctx, ctx[2]) if with_ctx_out else None
    return y_lat, y_ctx


def expert_mlps(t, expert, weight, w_gate, w_up, w_down):
    n_tok, d = t.shape
    m = expert.shape[0]
    token = jnp.arange(m, dtype=jnp.int32) // TOP_K
    order = jnp.argsort(expert)
    e_sorted = expert[order]
    counts = jax.ops.segment_sum(jnp.ones_like(expert), expert, num_segments=N_EXPERTS)
    padded = (counts + MOE_BLOCK - 1) // MOE_BLOCK * MOE_BLOCK
    pad_end = jnp.cumsum(padded)
    start = jnp.cumsum(counts) - counts
    slot = (pad_end - padded)[e_sorted] + jnp.arange(m, dtype=jnp.int32) - start[e_sorted]
    n_blocks = m // MOE_BLOCK + N_EXPERTS
    slot_token = jnp.full((n_blocks * MOE_BLOCK,), n_tok, jnp.int32).at[slot].set(token[order])
    slot_w = jnp.zeros((n_blocks * MOE_BLOCK,), weight.dtype).at[slot].set(weight[order])
    block_expert = jnp.clip(jnp.searchsorted(pad_end, jnp.arange(n_blocks) * MOE_BLOCK, side='right'), 0, N_EXPERTS - 1)
    t_pad = jnp.concatenate([t, jnp.zeros((1, d), t.dtype)], 0)

    def run_block(args):
        tok, e = args
        h = t_pad[tok]
        return (jax.nn.silu(h @ w_gate[e]) * (h @ w_up[e])) @ w_down[e]
    y = lax.map(run_block, (slot_token.reshape(n_blocks, MOE_BLOCK), block_expert)).reshape(-1, d)
    out = jnp.zeros((n_tok + 1, d), F32).at[slot_token].add(y.astype(F32) * slot_w[:, None])
    return out[:n_tok].astype(t.dtype)


def hier_moe(t, router_group_w, router_group_b, router_expert_w, router_expert_b, w_gate, w_up, w_down):
    n_tok = t.shape[0]
    tf = t.astype(F32)
    p_group = jax.nn.softmax(tf @ router_group_w + router_group_b, axis=-1)
    p_top_g, top_g = lax.top_k(p_group, 1)
    e_logits = (tf @ router_expert_w + router_expert_b).reshape(n_tok, N_GROUPS, EXPERTS_PER_GROUP)
    e_logits = e_logits[jnp.arange(n_tok), top_g[:, 0]]
    p_top_e, top_e = lax.top_k(jax.nn.softmax(e_logits, axis=-1), TOP_K)
    gate = p_top_g * p_top_e / jnp.sum(p_top_e, -1, keepdims=True)
    expert = (top_g * EXPERTS_PER_GROUP + top_e).reshape(-1)
    return expert_mlps(t, expert, gate.reshape(-1), w_gate, w_up, w_down)


def setup_inputs(seed: int = 0) -> dict:
    key = jax.random.key(seed)
    ks = iter(jax.random.split(key, 64))
    D = D_MODEL

    def nrm(shape, scale):
        return jax.random.normal(next(ks), shape, F32) * scale

    def unif(shape, lo, hi):
        return jax.random.uniform(next(ks), shape, F32, lo, hi)

    def dt_bias(shape):
        dt = jnp.exp(unif(shape, math.log(1e-3), math.log(1e-1)))
        return dt + jnp.log(-jnp.expm1(-dt))

    def logit_unif(shape, lo, hi):
        u = unif(shape, lo, hi)
        return jnp.log(u) - jnp.log1p(-u)

    return {
        'x': nrm((BATCH, SEQ, D), 1.0),
        'c': nrm((BATCH, D), 1.0),
        'ctx': nrm((BATCH, CTX_LEN, D), 1.0),
        'c_ctx': nrm((D,), 1.0),
        'hgrn_lb': nrm((DEPTH, HG_K), 0.5),
        'ada_w': nrm((DEPTH, D, 6 * D), 0.5 * D ** -0.5),
        'ada_b': nrm((DEPTH, 6 * D), 0.02),
        'ln_mix_g': 1.0 + nrm((DEPTH, D), 0.02),
        'ln_mix_b': nrm((DEPTH, D), 0.02),
        'ln_ffn_g': 1.0 + nrm((DEPTH, D), 0.02),
        'ln_ffn_b': nrm((DEPTH, D), 0.02),
        'mix_out_w': nrm((DEPTH, MIX_WIDTH, D), BETA * MIX_WIDTH ** -0.5),
        'router_group_w': nrm((DEPTH, D, N_GROUPS), D ** -0.5),
        'router_group_b': nrm((DEPTH, N_GROUPS), 0.01),
        'router_expert_w': nrm((DEPTH, D, N_EXPERTS), D ** -0.5),
        'router_expert_b': nrm((DEPTH, N_EXPERTS), 0.01),
        'exp_w_gate': nrm((DEPTH, N_EXPERTS, D, D_EXPERT), D ** -0.5),
        'exp_w_up': nrm((DEPTH, N_EXPERTS, D, D_EXPERT), D ** -0.5),
        'exp_w_down': nrm((DEPTH, N_EXPERTS, D_EXPERT, D), BETA * D_EXPERT ** -0.5),
        'ab_in_w': nrm((N_EVEN, D, AB_IN), D ** -0.5),
        'rg_conv_w': nrm((N_EVEN, CONV_W, RG_WIDTH), 0.5),
        'rg_conv_b': nrm((N_EVEN, RG_WIDTH), 0.02),
        'rg_gate_a_w': nrm((N_EVEN, 2, RG_HEADS, RG_BLOCK, RG_BLOCK), RG_BLOCK ** -0.5),
        'rg_gate_a_b': nrm((N_EVEN, 2, RG_WIDTH), 0.02),
        'rg_gate_x_w': nrm((N_EVEN, 2, RG_HEADS, RG_BLOCK, RG_BLOCK), RG_BLOCK ** -0.5),
        'rg_gate_x_b': nrm((N_EVEN, 2, RG_WIDTH), 0.02),
        'rg_lambda': logit_unif((N_EVEN, 2, RG_WIDTH), 0.9, 0.999),
        'ssd_conv_w': nrm((N_EVEN, CONV_W, SSD_CONV_DIM), 0.5),
        'ssd_conv_b': nrm((N_EVEN, SSD_CONV_DIM), 0.02),
        'ssd_a_log': jnp.log(unif((N_EVEN, 2, SSD_HEADS), 1.0, 16.0)),
        'ssd_dt_bias': dt_bias((N_EVEN, 2, SSD_HEADS)),
        'ssd_d': 1.0 + nrm((N_EVEN, SSD_HEADS), 0.02),
        'ssd_norm_g': 1.0 + nrm((N_EVEN, SSD_INNER), 0.02),
        'cd_in_w': nrm((N_ODD, D, CD_IN), D ** -0.5),
        'gdn_conv_w': nrm((N_ODD, CONV_W, 2 * GDN_QK + GDN_V), 0.5),
        'gdn_a_log': jnp.log(unif((N_ODD, 2, GDN_HEADS), 1.0, 16.0)),
        'gdn_dt_bias': dt_bias((N_ODD, 2, GDN_HEADS)),
        'gdn_norm_g': 1.0 + nrm((N_ODD, GDN_DV), 0.02),
        'hg_norm_g': 1.0 + nrm((N_ODD, HG_DV), 0.02),
    }


def reference(x, c, ctx, c_ctx, hgrn_lb, ada_w, ada_b, ln_mix_g, ln_mix_b, ln_ffn_g, ln_ffn_b, mix_out_w,
              router_group_w, router_group_b, router_expert_w, router_expert_b, exp_w_gate, exp_w_up, exp_w_down,
              ab_in_w, rg_conv_w, rg_conv_b, rg_gate_a_w, rg_gate_a_b, rg_gate_x_w, rg_gate_x_b, rg_lambda,
              ssd_conv_w, ssd_conv_b, ssd_a_log, ssd_dt_bias, ssd_d, ssd_norm_g,
              cd_in_w, gdn_conv_w, gdn_a_log, gdn_dt_bias, gdn_norm_g, hg_norm_g):
    rows = x.shape[1] // GRID_W
    d = x.shape[-1]
    lb_all = jnp.cumsum(jax.nn.softmax(hgrn_lb.astype(F32), axis=0), axis=0)
    lb_all = lb_all - lb_all[0]
    s_lat = jax.nn.silu(c)
    s_ctx = jax.nn.silu(c_ctx)
    h_lat, h_ctx = x, ctx
    for l in range(DEPTH):
        last = l == DEPTH - 1
        j = l // 2
        mod_lat = jnp.split((s_lat @ ada_w[l] + ada_b[l])[:, None, :], 6, axis=-1)
        mod_ctx = jnp.split(s_ctx @ ada_w[l] + ada_b[l], 6, axis=-1)
        u_lat = modulate(h_lat, mod_lat[0], mod_lat[1])
        u_ctx = modulate(h_ctx, mod_ctx[0], mod_ctx[1])
        if l % 2 == 0:
            y_lat, y_ctx = mixer_ab(u_lat, u_ctx, rows, ab_in_w[j], mix_out_w[l], rg_conv_w[j], rg_conv_b[j],
                                    rg_gate_a_w[j], rg_gate_a_b[j], rg_gate_x_w[j], rg_gate_x_b[j], rg_lambda[j],
                                    ssd_conv_w[j], ssd_conv_b[j], ssd_a_log[j], ssd_dt_bias[j], ssd_d[j],
                                    ssd_norm_g[j], not last)
        else:
            y_lat, y_ctx = mixer_cd(u_lat, u_ctx, rows, cd_in_w[j], mix_out_w[l], gdn_conv_w[j], gdn_a_log[j],
                                    gdn_dt_bias[j], gdn_norm_g[j], hg_norm_g[j], lb_all[l], not last)
        h_lat = layer_norm(ALPHA * h_lat + mod_lat[2] * y_lat, ln_mix_g[l], ln_mix_b[l])
        v_lat = modulate(h_lat, mod_lat[3], mod_lat[4]).reshape(-1, d)
        moe = lambda t: hier_moe(t, router_group_w[l], router_group_b[l], router_expert_w[l], router_expert_b[l],
                                 exp_w_gate[l], exp_w_up[l], exp_w_down[l])
        if last:
            f_lat = moe(v_lat)
        else:
            h_ctx = layer_norm(ALPHA * h_ctx + mod_ctx[2] * y_ctx, ln_mix_g[l], ln_mix_b[l])
            v_ctx = modulate(h_ctx, mod_ctx[3], mod_ctx[4]).reshape(-1, d)
            f_all = moe(jnp.concatenate([v_lat, v_ctx], 0))
            f_lat = f_all[:v_lat.shape[0]]
            h_ctx = layer_norm(ALPHA * h_ctx + mod_ctx[5] * f_all[v_lat.shape[0]:].reshape(h_ctx.shape),
                               ln_ffn_g[l], ln_ffn_b[l])
        h_lat = layer_norm(ALPHA * h_lat + mod_lat[5] * f_lat.reshape(h_lat.shape), ln_ffn_g[l], ln_ffn_b[l])
    return h_lat
```

```python
import numpy as np
from contextlib import ExitStack
import concourse.bass as bass
import concourse.mybir as mybir
from concourse.bass_utils import run_bass_kernel_spmd

F32 = mybir.dt.float32
BF16 = mybir.dt.bfloat16
AF = mybir.ActivationFunctionType
ALU = mybir.AluOpType
AX = mybir.AxisListType


class Buf:
    __slots__ = ("name", "w", "r", "psum")

    def __init__(self, name, psum=False):
        self.name = name
        self.w = None
        self.r = {}
        self.psum = psum


class V:
    __slots__ = ("ap", "bufs")

    def __init__(self, ap, bufs):
        self.ap = ap
        self.bufs = bufs

    def __getitem__(self, k):
        return V(self.ap[k], self.bufs)

    def re(self, pattern_, **kw):
        return V(self.ap.rearrange(pattern_, **kw), self.bufs)

    def bc(self, shape):
        return V(self.ap.to_broadcast(list(shape)), self.bufs)

    def pbc(self, shape):
        return V(self.ap.broadcast_to(list(shape)), self.bufs)

    def un(self, axis):
        return V(self.ap.unsqueeze(axis), self.bufs)

    def sub(self, name):
        return V(self.ap, (Buf(name),))


def _apof(x):
    return x.ap if isinstance(x, V) else x


def _bufsof(*xs):
    out = []
    for x in xs:
        if isinstance(x, V):
            out.extend(x.bufs)
    return out


class Prog:
    ENG = ["pe", "act", "dve", "pool", "sp"]
    RING = 8

    def __init__(self, nc):
        self.nc = nc
        self.es = ExitStack()
        self.eobj = {"pe": nc.tensor, "act": nc.scalar, "dve": nc.vector, "pool": nc.gpsimd, "sp": nc.sync}
        self.esem = {e: self.es.enter_context(nc.semaphore("s_" + e)) for e in self.ENG}
        self.cnt = {e: 0 for e in self.ENG}
        self.waited = {e: {} for e in self.ENG}
        self.ring = {}
        for e in ["sp", "pool"]:
            self.ring[e] = [[self.es.enter_context(nc.semaphore("d_%s%d" % (e, i))), 0] for i in range(self.RING)]
        self.ring_i = {e: 0 for e in self.ring}
        self.scopes = []
        self.ninst = 0

    def sb(self, name, shape, dtype=F32):
        es = self.scopes[-1] if self.scopes else self.es
        self.uid = getattr(self, "uid", 0) + 1
        name = "sb%d_%s" % (self.uid, name)
        t = es.enter_context(self.nc.sbuf_tensor(name, list(shape), dtype))
        return V(t[:], (Buf(name),))

    def ps(self, name, shape, dtype=F32):
        t = self.es.enter_context(self.nc.psum_tensor("ps_" + name, list(shape), dtype))
        return V(t[:], (Buf(name, psum=True),))

    def dram(self, name, shape, dtype=F32, kind="Internal"):
        t = self.nc.dram_tensor(name, list(shape), dtype, kind=kind)
        return V(t.ap(), (Buf(name),))

    def push(self):
        self.scopes.append(ExitStack())

    def pop(self):
        self.barrier()
        self.scopes.pop().close()

    def _wait(self, eng, tok):
        sem, val = tok
        k = id(sem)
        if self.waited[eng].get(k, 0) >= val:
            return
        self.eobj[eng].wait_ge(sem, val)
        self.waited[eng][k] = val
        self.ninst += 1

    def _deps(self, eng, reads, writes):
        toks = []
        mysem0 = self.esem.get(eng)
        for b in reads:
            if b.w is not None:
                toks.append(b.w)
            if b.psum:
                toks.extend(t for t in b.r.values() if t[0] is not mysem0)
        for b in writes:
            if b.w is not None:
                toks.append(b.w)
            toks.extend(b.r.values())
        mysem = self.esem.get(eng)
        for tok in toks:
            if eng == "pe" and tok[0] is mysem:
                continue
            self._wait(eng, tok)

    def _commit(self, tok, reads, writes):
        k = id(tok[0])
        for b in reads:
            b.r[k] = tok
        for b in writes:
            b.w = tok
            b.r = {}

    def op(self, eng, fn, reads, writes):
        self._deps(eng, reads, writes)
        inst = fn(self.eobj[eng])
        self.cnt[eng] += 1
        self.ninst += 1
        inst.then_inc(self.esem[eng], 1)
        tok = (self.esem[eng], self.cnt[eng])
        self._commit(tok, reads, writes)
        return tok

    def dma(self, eng, out, in_, **kw):
        reads = _bufsof(in_)
        writes = _bufsof(out)
        i = self.ring_i[eng]
        self.ring_i[eng] = (i + 1) % self.RING
        slot = self.ring[eng][i]
        if slot[1] > 0:
            self._wait(eng, (slot[0], slot[1]))
        self._deps(eng, reads, writes)
        self.eobj[eng].dma_start(out=_apof(out), in_=_apof(in_), **kw).then_inc(slot[0], 16)
        self.ninst += 1
        slot[1] += 16
        tok = (slot[0], slot[1])
        self._commit(tok, reads, writes)
        return tok

    def collective(self, kind, out, in_, groups):
        eng = "pool"
        reads = _bufsof(in_)
        writes = _bufsof(out)
        if not hasattr(self, "ccsem"):
            self.ccsem = self.es.enter_context(self.nc.semaphore("s_cc"))
            self.cccnt = 0
        self._deps(eng, reads, writes)
        for slot in self.ring[eng]:
            if slot[1] > 0:
                self._wait(eng, (slot[0], slot[1]))
        self.nc.gpsimd.collective_compute(kind, ALU.bypass, replica_groups=groups, ins=[_apof(in_)],
                                          outs=[_apof(out)]).then_inc(self.ccsem, 1)
        self.ninst += 1
        self.cccnt += 1
        tok = (self.ccsem, self.cccnt)
        self._commit(tok, reads, writes)
        self._wait(eng, tok)
        return tok

    def barrier(self):
        toks = [(self.esem[e], self.cnt[e]) for e in self.ENG if self.cnt[e] > 0]
        for e in self.ring:
            for slot in self.ring[e]:
                if slot[1] > 0:
                    toks.append((slot[0], slot[1]))
        if getattr(self, "cccnt", 0) > 0:
            toks.append((self.ccsem, self.cccnt))
        for e in self.ENG:
            for tok in toks:
                if tok[0] is self.esem[e]:
                    continue
                self._wait(e, tok)

    def finish(self):
        self.barrier()
        while self.scopes:
            self.scopes.pop().close()
        self.es.close()

    def mm(self, out, lhsT, rhs, start=True, stop=True):
        return self.op("pe", lambda e: e.matmul(_apof(out), lhsT=_apof(lhsT), rhs=_apof(rhs), start=start, stop=stop),
                       _bufsof(lhsT, rhs), _bufsof(out))

    def tr(self, out, in_, ident):
        return self.op("pe", lambda e: e.transpose(out=_apof(out), in_=_apof(in_), identity=_apof(ident)),
                       _bufsof(in_, ident), _bufsof(out))

    def act(self, out, in_, func, bias=None, scale=None, accum=None):
        kw = {}
        if bias is not None:
            kw["bias"] = _apof(bias)
        if scale is not None:
            kw["scale"] = _apof(scale)
        if accum is not None:
            kw["accum_out"] = _apof(accum)
        return self.op("act", lambda e: e.activation(out=_apof(out), in_=_apof(in_), func=func, **kw),
                       _bufsof(in_, bias, scale), _bufsof(out, accum))

    def tt(self, eng, out, in0, in1, op):
        return self.op(eng, lambda e: e.tensor_tensor(out=_apof(out), in0=_apof(in0), in1=_apof(in1), op=op),
                       _bufsof(in0, in1), _bufsof(out))

    def ts(self, eng, out, in0, s1, s2, op0, op1=None, accum=None):
        kw = {}
        if op1 is not None:
            kw["op1"] = op1
        if accum is not None:
            kw["accum_out"] = _apof(accum)
        return self.op(eng, lambda e: e.tensor_scalar(out=_apof(out), in0=_apof(in0), scalar1=_apof(s1),
                                                      scalar2=_apof(s2), op0=op0, **kw),
                       _bufsof(in0, s1, s2), _bufsof(out, accum))

    def stt(self, eng, out, in0, scalar, in1, op0, op1):
        return self.op(eng, lambda e: e.scalar_tensor_tensor(out=_apof(out), in0=_apof(in0), scalar=_apof(scalar),
                                                             in1=_apof(in1), op0=op0, op1=op1),
                       _bufsof(in0, scalar, in1), _bufsof(out))

    def cp(self, eng, out, in_):
        if eng == "act":
            return self.act(out, in_, AF.Copy)
        return self.op(eng, lambda e: e.tensor_copy(out=_apof(out), in_=_apof(in_)), _bufsof(in_), _bufsof(out))

    def memset(self, eng, out, val):
        return self.op(eng, lambda e: e.memset(_apof(out), val), [], _bufsof(out))

    def scan(self, out, d0, d1, init, op0=ALU.mult, op1=ALU.add):
        return self.op("dve", lambda e: e.tensor_tensor_scan(out=_apof(out), data0=_apof(d0), data1=_apof(d1),
                                                             initial=_apof(init), op0=op0, op1=op1),
                       _bufsof(d0, d1, init), _bufsof(out))

    def red(self, eng, out, in_, op, axis=AX.X):
        return self.op(eng, lambda e: e.tensor_reduce(out=_apof(out), in_=_apof(in_), axis=axis, op=op),
                       _bufsof(in_), _bufsof(out))

    def recip(self, out, in_):
        return self.op("dve", lambda e: e.reciprocal(out=_apof(out), in_=_apof(in_)), _bufsof(in_), _bufsof(out))
D = 1024
NCTX = 256
NLAT = 4096
NT = NCTX + NLAT
ALPHA_RES = 4.0 ** 0.25
LN_EPS = 1e-5
RMS_EPS = 1e-6
AB_IN = 4640
CD_IN = 9248

C_ID, C_ONES, C_MLE, C_MLT, C_MGE, C_MGT, C_RESET = 0, 128, 256, 384, 512, 640, 768
NCONST = 768 + 576


def make_consts():
    c = np.zeros((128, NCONST), np.float32)
    s = np.arange(128)[:, None]
    t = np.arange(128)[None, :]
    c[:, C_ID:C_ID + 128] = (s == t)
    c[:, C_ONES:C_ONES + 128] = 1.0
    c[:, C_MLE:C_MLE + 128] = (s <= t)
    c[:, C_MLT:C_MLT + 128] = (s < t)
    c[:, C_MGE:C_MGE + 128] = (s >= t)
    c[:, C_MGT:C_MGT + 128] = (s > t)
    c[:, C_RESET:C_RESET + 576] = (np.arange(576) % 64 != 0)[None, :]
    return c


class Layout:
    def __init__(self):
        self.off = {}
        self.n = 0

    def add(self, name, n):
        self.off[name] = (self.n, n)
        self.n += n

    def sl(self, name):
        o, n = self.off[name]
        return slice(o, o + n)


def make_layouts():
    pc = Layout()
    pc.add("cvec", 16)
    for l in range(2):
        pc.add("adab%d" % l, 48)
    pc.add("rgconv", 40); pc.add("rgconvb", 8); pc.add("rgba", 16); pc.add("rgbx", 16); pc.add("rglam", 16)
    pc.add("ssdconv", 60); pc.add("ssdconvb", 12)
    pc.add("gdnconv", 120); pc.add("hglb0", 8); pc.add("hglb1", 8)
    pc.add("psel", 8)
    pv = Layout()
    for l in range(2):
        pv.add("adabrow%d" % l, 2048)
        for nm in ("lnmg", "lnmb", "lnfg", "lnfb"):
            pv.add("%s%d" % (nm, l), 1024)
        pv.add("rb%d" % l, 36)
    pv.add("dtbias", 32); pv.add("alog", 32); pv.add("ssdd", 1024); pv.add("ssdng", 1024)
    pv.add("galog", 16); pv.add("gdtb", 16); pv.add("gdnng", 1024); pv.add("hgng", 1024)
    return pc, pv


PC, PV = make_layouts()


def colmajor(vec):
    v = np.asarray(vec, np.float32).reshape(-1, 128)
    return np.ascontiguousarray(v.T)


def conv5(w, rev):
    w = np.asarray(w, np.float32)
    z = np.zeros((1, w.shape[1]), np.float32)
    if not rev:
        return np.concatenate([w, z], 0)
    return np.concatenate([z, w[::-1]], 0)


def conv_cols(w5):
    C = w5.shape[1]
    a = w5.reshape(5, C // 128, 128)
    return np.ascontiguousarray(a.transpose(2, 1, 0).reshape(128, -1))


def prep_shared(inp):
    sh = {}
    ab = inp["ab_in_w"][0]
    cd = inp["cd_in_w"][0]
    ab_odd = np.concatenate([ab[:, :4608], ab[:, 4624:4640], ab[:, 4608:4624]], 1)
    cd_odd = np.concatenate([cd[:, :4096], cd[:, 4104:4112], cd[:, 4096:4104], cd[:, 4120:4128], cd[:, 4112:4120],
                             cd[:, 4128:5152], cd[:, 6176:7200], cd[:, 5152:6176], cd[:, 7200:]], 1)
    sh["ab_w"] = [np.ascontiguousarray(ab), np.ascontiguousarray(ab_odd)]
    sh["cd_w"] = [np.ascontiguousarray(cd), np.ascontiguousarray(cd_odd)]
    ga = inp["rg_gate_a_w"][0]
    gx = inp["rg_gate_x_w"][0]
    sh["rg_gw"] = []
    for par in range(2):
        do = [1, 0] if par else [0, 1]
        sh["rg_gw"].append(np.ascontiguousarray(np.stack([np.stack([ga[d], gx[d]], 0) for d in do], 0)))
    sh["router_w"] = np.ascontiguousarray(np.concatenate([inp["router_group_w"], inp["router_expert_w"]], -1))
    sh["consts"] = make_consts()
    return sh


def prep_core(inp, b, half, core_index=0):
    rev = half == 1
    do = [1, 0] if rev else [0, 1]
    x = inp["x"][b]
    cx = inp["ctx"][b]
    lat = x[NLAT:][::-1] if rev else x[:NLAT]
    cxx = cx[::-1] if rev else cx
    h0 = np.ascontiguousarray(np.concatenate([cxx, lat], 0).astype(np.float32))
    pc = np.zeros((128, PC.n), np.float32)
    cv = np.stack([inp["c"][b], inp["c_ctx"]], 0).reshape(2, 8, 128)
    pc[:, PC.sl("cvec")] = cv.transpose(2, 1, 0).reshape(128, 16)
    for l in range(2):
        pc[:, PC.sl("adab%d" % l)] = colmajor(inp["ada_b"][l])
    pc[:, PC.sl("rgconv")] = conv_cols(conv5(inp["rg_conv_w"][0], rev))
    pc[:, PC.sl("rgconvb")] = colmajor(inp["rg_conv_b"][0])
    pc[:, PC.sl("rgba")] = np.concatenate([colmajor(inp["rg_gate_a_b"][0][d]) for d in do], 1)
    pc[:, PC.sl("rgbx")] = np.concatenate([colmajor(inp["rg_gate_x_b"][0][d]) for d in do], 1)
    pc[:, PC.sl("rglam")] = np.concatenate([colmajor(inp["rg_lambda"][0][d]) for d in do], 1)
    pc[:, PC.sl("ssdconv")] = conv_cols(conv5(inp["ssd_conv_w"][0], rev))
    pc[:, PC.sl("ssdconvb")] = colmajor(inp["ssd_conv_b"][0])
    pc[:, PC.sl("gdnconv")] = conv_cols(conv5(inp["gdn_conv_w"][0], rev))
    pc[:, PC.sl("hglb0")] = colmajor(inp["hgrn_lb"][0])
    pc[:, PC.sl("hglb1")] = colmajor(inp["hgrn_lb"][1])
    psel = np.zeros((128, 8), np.float32)
    psel[:, core_index ^ 1] = 1.0
    pc[:, PC.sl("psel")] = psel
    pv = np.zeros((PV.n,), np.float32)
    for l in range(2):
        ab = inp["ada_b"][l]
        pv[PV.sl("adabrow%d" % l)] = np.concatenate([ab[2048:3072], ab[5120:6144]])
        pv[PV.sl("lnmg%d" % l)] = inp["ln_mix_g"][l]
        pv[PV.sl("lnmb%d" % l)] = inp["ln_mix_b"][l]
        pv[PV.sl("lnfg%d" % l)] = inp["ln_ffn_g"][l]
        pv[PV.sl("lnfb%d" % l)] = inp["ln_ffn_b"][l]
        pv[PV.sl("rb%d" % l)] = np.concatenate([inp["router_group_b"][l], inp["router_expert_b"][l]])
    pv[PV.sl("dtbias")] = np.concatenate([inp["ssd_dt_bias"][0][d] for d in do])
    pv[PV.sl("alog")] = np.concatenate([inp["ssd_a_log"][0][d] for d in do])
    pv[PV.sl("ssdd")] = np.repeat(inp["ssd_d"][0], 64)
    pv[PV.sl("ssdng")] = inp["ssd_norm_g"][0]
    pv[PV.sl("galog")] = np.concatenate([inp["gdn_a_log"][0][d] for d in do])
    pv[PV.sl("gdtb")] = np.concatenate([inp["gdn_dt_bias"][0][d] for d in do])
    pv[PV.sl("gdnng")] = np.tile(inp["gdn_norm_g"][0], 8)
    pv[PV.sl("hgng")] = np.tile(inp["hg_norm_g"][0], 8)
    return {"h0": h0, "pcol": pc, "pvec": pv.reshape(1, -1)}
class KB:
    def __init__(self):
        self.nc = bass.Bass("TRN2", target_bir_lowering=False)
        self.P = Prog(self.nc)
        self.d = {}
        self.outs = []

    def ext_in(self, name, shape, dtype=F32):
        v = self.P.dram(name, shape, dtype, kind="ExternalInput")
        self.d[name] = v
        return v

    def ext_out(self, name, shape, dtype=F32):
        v = self.P.dram(name, shape, dtype, kind="ExternalOutput")
        self.d[name] = v
        self.outs.append(name)
        return v

    def scratch(self, name, shape, dtype=F32):
        v = self.P.dram(name, shape, dtype, kind="Internal")
        self.d[name] = v
        return v

    def setup(self):
        P = self.P
        self.pb = []
        self.slots = []
        for i in range(8):
            bank = P.ps("bank%d" % i, [128, 512])
            self.pb.append(bank)
            self.slots.append([V(bank.ap[:, q * 128:(q + 1) * 128], bank.bufs) for q in range(4)])
        self.cst = P.sb("cst_sb", [128, NCONST])
        P.dma("sp", self.cst, self.d["consts"])
        self.pcol = P.sb("pcol_sb", [128, PC.n])
        P.dma("sp", self.pcol, self.d["pcol"])
        self.pvec = self.d["pvec"]
        c = self.cst
        self.ident = c[:, C_ID:C_ID + 128]
        self.ones = c[:, C_ONES:C_ONES + 128]
        self.MLE = c[:, C_MLE:C_MLE + 128]
        self.MLT = c[:, C_MLT:C_MLT + 128]
        self.MGE = c[:, C_MGE:C_MGE + 128]
        self.MGT = c[:, C_MGT:C_MGT + 128]
        self.RESET = c[:, C_RESET:C_RESET + 576]
        self.onesb = P.sb("onesb", [128, 128], BF16)
        P.cp("dve", self.onesb, self.ones)
        self.modc = [P.sb("modc%d" % l, [128, 6, 8, 2]) for l in range(2)]
        self.hin = [P.sb("hin%d" % i, [128, 1024]) for i in range(2)]
        self.hin_i = 0

    def pcs(self, name, i=None, n=1):
        o, _ = PC.off[name]
        if i is None:
            return self.pcol[:, PC.sl(name)]
        return self.pcol[:, o + i:o + i + n]

    def bcast_row(self, name, tile, n=None):
        o, nn = PV.off[name]
        n = nn if n is None else n
        self.P.dma("sp", tile, self.pvec[0:1, o:o + n].pbc([128, n]))

    def phase_mod(self, l):
        P = self.P
        modc = self.modc[l]
        P.push()
        sT = P.sb("sT", [128, 8, 2])
        P.act(sT.re("p k r -> p (k r)"), self.pcs("cvec"), AF.Silu)
        rowsb = P.sb("rowsb", [2, 2048])
        adabrow = P.sb("adabrow", [2, 2048])
        o, n = PV.off["adabrow%d" % l]
        P.dma("sp", adabrow, self.pvec[0:1, o:o + n].pbc([2, n]))
        wt = [P.sb("adaw%d" % i, [128, 8, 512]) for i in range(2)]
        adaw = self.d["ada_w"]
        ob, _ = PC.off["adab%d" % l]
        for cb in range(12):
            w = wt[cb % 2]
            P.dma("sp", w, adaw[l, :, cb * 512:(cb + 1) * 512].re("(k p) c -> p k c", p=128))
            j = cb // 2
            if j in (2, 5):
                jj = 0 if j == 2 else 1
                ps = self.pb[cb % 2]
                for k in range(8):
                    P.mm(ps[0:2, :], sT[:, k, :], w[:, k, :], start=(k == 0), stop=(k == 7))
                sl = slice(jj * 1024 + (cb % 2) * 512, jj * 1024 + (cb % 2) * 512 + 512)
                P.tt("dve", rowsb[:, sl], ps[0:2, :], adabrow[:, sl], ALU.add)
            else:
                ps = self.pb[2 + cb % 2]
                for cc in range(4):
                    for k in range(8):
                        P.mm(ps[:, cc * 2:cc * 2 + 2], w[:, k, cc * 128:(cc + 1) * 128], sT[:, k, :],
                             start=(k == 0), stop=(k == 7))
                for cc in range(4):
                    kk = (cb % 2) * 4 + cc
                    P.ts("dve", modc[:, j, kk, :], ps[:, cc * 2:cc * 2 + 2],
                         self.pcol[:, ob + j * 8 + kk:ob + j * 8 + kk + 1], None, ALU.add)
        for j in (1, 4):
            P.ts("dve", modc[:, j, :, :], modc[:, j, :, :], 1.0, None, ALU.add)
        P.dma("sp", self.d["modrow%d" % l], rowsb)
        P.pop()

    def make_uT(self, l, hsrc, tok0, ntok, r, jsh, jsc, uT, u32=None):
        P = self.P
        modc = self.modc[l]
        for jb in range(ntok // 128):
            hin = self.hin[self.hin_i % 2]
            self.hin_i += 1
            P.dma("sp", hin, hsrc[tok0 + jb * 128:tok0 + (jb + 1) * 128, :])
            for half in range(2):
                ps = self.pb[half]
                for q in range(4):
                    k = half * 4 + q
                    P.tr(ps[:, q * 128:(q + 1) * 128], hin[:, k * 128:(k + 1) * 128], self.ident)
                for q in range(4):
                    k = half * 4 + q
                    P.act(uT[:, k, jb * 128:(jb + 1) * 128], ps[:, q * 128:(q + 1) * 128], AF.Identity,
                          bias=modc[:, jsh, k, r:r + 1], scale=modc[:, jsc, k, r:r + 1])
                    if u32 is not None:
                        P.act(u32[:, k, jb * 128:(jb + 1) * 128], ps[:, q * 128:(q + 1) * 128], AF.Identity,
                              bias=modc[:, jsh, k, r:r + 1], scale=modc[:, jsc, k, r:r + 1])

    def conv(self, out, src, wcols, bias, ntok, seg):
        P = self.P
        o2 = out[:, :ntok]
        s2 = src[:, :ntok]
        if bias is not None:
            P.ts("dve", o2, s2, wcols[:, 2:3], bias, ALU.mult, ALU.add)
        else:
            P.ts("dve", o2, s2, wcols[:, 2:3], None, ALU.mult)
        o3 = o2.re("p (r s) -> p r s", s=seg)
        s3 = s2.re("p (r s) -> p r s", s=seg)
        for o in (-2, -1, 1, 2):
            lo_o, hi_o = max(0, -o), seg - max(0, o)
            lo_i, hi_i = max(0, o), seg - max(0, -o)
            P.stt("dve", o3[:, :, lo_o:hi_o], s3[:, :, lo_i:hi_i], wcols[:, o + 2:o + 3], o3[:, :, lo_o:hi_o],
                  ALU.mult, ALU.add)

    def load_w(self, dst, wsrc, c0, ncol):
        step = 512
        for a in range(0, ncol, step):
            n = min(step, ncol - a)
            self.P.dma("pool", dst[:, :, a:a + n], wsrc[:, c0 + a:c0 + a + n].re("(k p) c -> p k c", p=128))

    def mixer_ab_begin(self, d):
        P = self.P
        P.push()
        W = self.d["ab_w"]
        self.Wrg = P.sb("Wrg", [128, 8, 1024], BF16)
        self.load_w(self.Wrg, W, 0, 1024)
        self.Wxbc = P.sb("Wxbc", [128, 8, 1536], BF16)
        self.load_w(self.Wxbc, W, 3072, 1536)
        self.Wdt = P.sb("Wdt", [128, 8, 16], BF16)
        self.load_w(self.Wdt, W, 4608 + 16 * d, 16)
        self.Wg = P.sb("Wg", [128, 2, 8, 128], BF16)
        P.dma("pool", self.Wg, self.d["rg_gw"][d].re("g h i j -> i g h j"))
        self.spl = P.sb("spl", [128, 16])
        P.act(self.spl, self.pcs("rglam"), AF.Exp, scale=-1.0)
        P.act(self.spl, self.spl, AF.Ln, bias=1.0)
        P.ts("dve", self.spl, self.spl, -8.0, None, ALU.mult)
        self.negA = P.sb("negA", [128, 32])
        self.bcast_row("alog", self.negA)
        P.act(self.negA, self.negA, AF.Exp)
        P.ts("dve", self.negA, self.negA, -1.0, None, ALU.mult)
        self.dtb = P.sb("dtb", [128, 32])
        self.bcast_row("dtbias", self.dtb)
        self.ssdd = P.sb("ssdd", [128, 1024])
        self.bcast_row("ssdd", self.ssdd)
        self.rgst = P.sb("rgst", [128, 8])
        self.S = P.sb("S", [128, 1024])
        self.Sbf = P.sb("Sbf", [128, 1024], BF16)
        self.uT = P.sb("uT", [128, 8, 512], BF16)
        self.xbcT = P.sb("xbcT", [128, 12, 512])
        self.BTb = P.sb("BTb", [128, 2, 512], BF16)
        self.CTb = P.sb("CTb", [128, 2, 512], BF16)
        nm = ["xc", "ra", "ix", "a2", "hT"]
        self.rgts = [{n: P.sb("rg_%s%d" % (n, i), [128, 512]) for n in nm} for i in range(2)]
        self.xcbs = [P.sb("xcb%d" % i, [128, 512], BF16) for i in range(2)]
        self.xts = [P.sb("xts%d" % i, [128, 512]) for i in range(3)]
        self.t_dt = P.sb("t_dt", [128, 16])
        self.t_la = P.sb("t_la", [128, 16])
        self.t_E = P.sb("t_E", [128, 48])
        self.t_V = P.sb("t_V", [128, 16, 128])
        self.t_dec = P.sb("t_dec", [128, 16, 128], BF16)
        self.t_sm = P.sb("t_sm", [128, 2, 128], BF16)
        self.t_MT = P.sb("t_MT", [128, 16, 128], BF16)
        self.t_xs = P.sb("t_xs", [128, 1024])
        self.t_xdt = P.sb("t_xdt", [128, 1024], BF16)
        self.t_xw = P.sb("t_xw", [128, 1024], BF16)
        self.t_Bt = P.sb("t_Bt", [128, 256], BF16)
        self.t_y = P.sb("t_y", [128, 1024])
        self.t_tmp = P.sb("t_tmp", [128, 1024])

    def mixer_state_init(self, st_in):
        P = self.P
        if st_in is None:
            P.memset("dve", self.rgst, 0.0)
            P.memset("dve", self.S, 0.0)
        else:
            P.dma("sp", self.S, st_in[0])
            P.dma("sp", self.rgst, st_in[1][:, 0:8])
        P.cp("act", self.Sbf, self.S)

    def mixer_state_out(self, st_out):
        P = self.P
        P.dma("sp", st_out[0], self.S)
        z = self.t_tmp
        P.memset("dve", z, 0.0)
        P.cp("dve", z[:, 0:8], self.rgst)
        P.dma("sp", st_out[1], z)

    def mixer_end(self):
        self.P.pop()

    def mixer_ab_tile(self, d, tile, o_rg, o_ssd, add_skip, hsrc):
        P = self.P
        tok0, ntok, seg, is_ctx = tile
        dirB = d == 1
        r = 1 if is_ctx else 0
        uT = self.uT
        self.make_uT(0, hsrc, tok0, ntok, r, 0, 1, uT)
        for hh in range(8):
            T = self.rgts[hh % 2]
            xcb_ = self.xcbs[hh % 2]
            ps = self.pb[2 + hh % 2]
            for k in range(8):
                P.mm(ps[:, :ntok], self.Wrg[:, k, hh * 128:(hh + 1) * 128], uT[:, k, :ntok], start=(k == 0), stop=(k == 7))
            xc = T["xc"]
            self.conv(xc, ps, self.pcs("rgconv", hh * 5, 5), self.pcs("rgconvb", hh), ntok, seg)
            P.cp("act", xcb_[:, :ntok], xc[:, :ntok])
            psa, psx = (self.pb[4], self.pb[5]) if hh % 2 == 0 else (self.pb[6], self.pb[7])
            P.mm(psa[:, :ntok], self.Wg[:, 0, hh, :], xcb_[:, :ntok])
            P.mm(psx[:, :ntok], self.Wg[:, 1, hh, :], xcb_[:, :ntok])
            ra, ix, a2, hT = T["ra"], T["ix"], T["a2"], T["hT"]
            P.act(ra[:, :ntok], psa[:, :ntok], AF.Sigmoid, bias=self.pcs("rgba", d * 8 + hh))
            P.act(ix[:, :ntok], psx[:, :ntok], AF.Sigmoid, bias=self.pcs("rgbx", d * 8 + hh))
            P.act(ra[:, :ntok], ra[:, :ntok], AF.Exp, scale=self.spl[:, d * 8 + hh:d * 8 + hh + 1])
            P.tt("dve", a2[:, :ntok], ra[:, :ntok], ra[:, :ntok], ALU.mult)
            P.act(a2[:, :ntok], a2[:, :ntok], AF.Sqrt, bias=1.0, scale=-1.0)
            P.tt("dve", ix[:, :ntok], ix[:, :ntok], xc[:, :ntok], ALU.mult)
            P.tt("dve", ix[:, :ntok], ix[:, :ntok], a2[:, :ntok], ALU.mult)
            st = self.rgst[:, hh:hh + 1]
            if not dirB:
                P.scan(hT[:, :ntok], ra[:, :ntok], ix[:, :ntok], st)
                P.cp("dve", st, hT[:, ntok - 1:ntok])
            else:
                P.scan(hT[:, :ntok][:, ::-1], ra[:, :ntok][:, ::-1], ix[:, :ntok][:, ::-1], st)
                P.cp("dve", st, hT[:, 0:1])
            P.dma("sp", o_rg[hh * 128:(hh + 1) * 128, tok0:tok0 + ntok], hT[:, :ntok])
        for cc in range(12):
            xt = self.xts[cc % 3]
            ps = self.pb[2 + cc % 4]
            for k in range(8):
                P.mm(ps[:, :ntok], self.Wxbc[:, k, cc * 128:(cc + 1) * 128], uT[:, k, :ntok], start=(k == 0), stop=(k == 7))
            self.conv(xt, ps, self.pcs("ssdconv", cc * 5, 5), self.pcs("ssdconvb", cc), ntok, seg)
            P.act(self.xbcT[:, cc, :ntok], xt[:, :ntok], AF.Silu)
            if cc in (8, 9):
                P.cp("dve", self.BTb[:, cc - 8, :ntok], self.xbcT[:, cc, :ntok])
            if cc in (10, 11):
                P.cp("dve", self.CTb[:, cc - 10, :ntok], self.xbcT[:, cc, :ntok])
        nch = ntok // 128
        order = range(nch - 1, -1, -1) if dirB else range(nch)
        for ci in order:
            self.ssd_chunk(d, ci * 128, tok0 + ci * 128, o_ssd, add_skip)

    def ssd_chunk(self, d, c0, tok_abs, o_ssd, add_skip):
        P = self.P
        pb = self.pb
        dirB = d == 1
        cs = slice(c0, c0 + 128)
        M1 = self.MLT if dirB else self.MGT
        M2 = self.MGE if dirB else self.MLE
        uT = self.uT
        for k in range(8):
            P.mm(pb[6][:, 0:16], uT[:, k, cs], self.Wdt[:, k, :], start=(k == 0), stop=(k == 7))
        dt, la, E = self.t_dt, self.t_la, self.t_E
        P.tt("dve", dt, pb[6][:, 0:16], self.dtb[:, d * 16:(d + 1) * 16], ALU.add)
        P.act(dt, dt, AF.Exp)
        P.act(dt, dt, AF.Ln, bias=1.0)
        P.tt("dve", la, dt, self.negA[:, d * 16:(d + 1) * 16], ALU.mult)
        P.mm(pb[5][:, 0:16], M2, la)
        P.mm(pb[5][:, 16:32], M1, la)
        P.mm(pb[5][:, 32:48], self.ones, la)
        P.act(E, pb[5][:, 0:48], AF.Exp)
        ecum, wend, etot = E[:, 0:16], E[:, 16:32], E[:, 32:48]
        Vt = self.t_V
        P.tt("dve", Vt, M1.un(1).bc([128, 16, 128]), la.un(2).bc([128, 16, 128]), ALU.mult)
        for r in range(16):
            P.mm(pb[r // 4][:, (r % 4) * 128:(r % 4 + 1) * 128], Vt[:, r, :], M2)
        dec = self.t_dec
        for q in range(4):
            P.act(dec[:, q * 4:(q + 1) * 4, :].re("p a b -> p (a b)"), pb[q], AF.Exp)
        for g in range(2):
            P.mm(pb[4][:, g * 128:(g + 1) * 128], self.BTb[:, g, cs], self.CTb[:, g, cs])
        sm = self.t_sm
        P.tt("dve", sm, pb[4][:, 0:256].re("p (g t) -> p g t", g=2), M2.un(1).bc([128, 2, 128]), ALU.mult)
        MT = self.t_MT
        for g in range(2):
            P.tt("dve", MT[:, g * 8:(g + 1) * 8, :], dec[:, g * 8:(g + 1) * 8, :], sm[:, g, :].un(1).bc([128, 8, 128]), ALU.mult)
        for half in range(2):
            for q in range(4):
                P.tr(pb[6 + half][:, q * 128:(q + 1) * 128], self.xbcT[:, half * 4 + q, cs], self.ident)
        xs = self.t_xs
        P.cp("act", xs[:, 0:512], pb[6])
        P.cp("act", xs[:, 512:1024], pb[7])
        for g in range(2):
            P.tr(pb[5][:, 128 + g * 128:256 + g * 128], self.xbcT[:, 8 + g, cs], self.ident)
        P.cp("dve", self.t_Bt, pb[5][:, 128:384])
        xdt, xw = self.t_xdt, self.t_xw
        P.tt("dve", xdt.re("p (r c) -> p r c", c=64), xs.re("p (r c) -> p r c", c=64), dt.un(2).bc([128, 16, 64]), ALU.mult)
        P.tt("dve", xw.re("p (r c) -> p r c", c=64), xdt.re("p (r c) -> p r c", c=64), wend.un(2).bc([128, 16, 64]), ALU.mult)
        for r in range(16):
            P.mm(pb[4 + r // 8][:, (r % 8) * 64:(r % 8 + 1) * 64], MT[:, r, :], xdt[:, r * 64:(r + 1) * 64])
        for g in range(2):
            P.mm(pb[6 + g], self.CTb[:, g, cs], self.Sbf[:, g * 512:(g + 1) * 512])
        y, tmp = self.t_y, self.t_tmp
        P.cp("act", y[:, 0:512], pb[4])
        P.cp("act", y[:, 512:1024], pb[5])
        for g in range(2):
            P.tt("dve", tmp[:, g * 512:(g + 1) * 512].re("p (r c) -> p r c", c=64), pb[6 + g].re("p (r c) -> p r c", c=64),
                 ecum[:, g * 8:(g + 1) * 8].un(2).bc([128, 8, 64]), ALU.mult)
        P.tt("dve", y, y, tmp, ALU.add)
        if add_skip:
            P.tt("dve", tmp, xs, self.ssdd, ALU.mult)
            P.tt("dve", y, y, tmp, ALU.add)
        P.dma("sp", o_ssd[tok_abs:tok_abs + 128, :], y)
        for g in range(2):
            P.mm(pb[g], self.t_Bt[:, g * 128:(g + 1) * 128], xw[:, g * 512:(g + 1) * 512])
        S = self.S
        P.tt("dve", S.re("p (r c) -> p r c", c=64), S.re("p (r c) -> p r c", c=64), etot.un(2).bc([128, 16, 64]), ALU.mult)
        for g in range(2):
            P.tt("dve", S[:, g * 512:(g + 1) * 512], S[:, g * 512:(g + 1) * 512], pb[g], ALU.add)
        P.cp("act", self.Sbf, S)


def tiles_all():
    t = [(0, 256, 256, True)]
    for i in range(8):
        t.append((NCTX + i * 512, 512, 64, False))
    return t
class KB2(KB):
    def ln_rows(self, x, g_bc, b_bc, out, tmp):
        P = self.P
        st = self.ln_st
        P.act(tmp, x, AF.Identity, accum=st[:, 0:1])
        P.act(tmp, x, AF.Square, accum=st[:, 1:2])
        P.ts("dve", st[:, 2:3], st[:, 0:1], 1.0 / 1024, None, ALU.mult)
        P.tt("dve", st[:, 3:4], st[:, 2:3], st[:, 2:3], ALU.mult)
        P.stt("dve", st[:, 4:5], st[:, 1:2], 1.0 / 1024, st[:, 3:4], ALU.mult, ALU.subtract)
        P.act(st[:, 5:6], st[:, 4:5], AF.Sqrt, bias=self.epsln[:, 0:1])
        P.recip(st[:, 6:7], st[:, 5:6])
        P.ts("dve", tmp, x, st[:, 2:3], st[:, 6:7], ALU.subtract, ALU.mult)
        P.tt("dve", tmp, tmp, g_bc, ALU.mult)
        P.tt("dve", out, tmp, b_bc, ALU.add)

    def small_consts(self):
        P = self.P
        self.ln_st = P.sb("ln_st", [128, 8])
        self.epsln = P.sb("epsln", [128, 2])
        P.memset("dve", self.epsln[:, 0:1], LN_EPS)
        P.memset("dve", self.epsln[:, 1:2], RMS_EPS)

    def phase_out(self, l, tiles, hsrc, hdst, oA, oB):
        P = self.P
        pb = self.pb
        P.push()
        self.small_consts()
        Wo = P.sb("Wo", [128, 16, 1024], BF16)
        mo = self.d["mix_out_w"]
        for c4 in range(4):
            P.dma("pool", Wo[:, c4 * 4:(c4 + 1) * 4, :], mo[l, c4 * 512:(c4 + 1) * 512, :].re("(c p) d -> p c d", p=128))
        Wg2 = P.sb("Wg2", [128, 8, 2048], BF16)
        if l == 0:
            self.load_w(Wg2, self.d["ab_w"], 1024, 2048)
        else:
            self.load_w(Wg2[:, :, 0:1024], self.d["cd_w"], 3072, 1024)
            self.load_w(Wg2[:, :, 1024:2048], self.d["cd_w"], 8224, 1024)
        gmix = [P.sb("gmix%d" % r, [128, 1024]) for r in range(2)]
        for r in range(2):
            P.dma("sp", gmix[r], self.d["modrow%d" % l][r:r + 1, 0:1024].pbc([128, 1024]))
        lng = P.sb("lng", [128, 1024]); self.bcast_row("lnmg%d" % l, lng)
        lnb = P.sb("lnb", [128, 1024]); self.bcast_row("lnmb%d" % l, lnb)
        if l == 0:
            ng = P.sb("ng", [128, 1024]); self.bcast_row("ssdng", ng)
        else:
            ng = P.sb("ng", [128, 2048])
            self.bcast_row("gdnng", ng[:, 0:1024]); self.bcast_row("hgng", ng[:, 1024:2048])
        uT = P.sb("uT", [128, 8, 512], BF16)
        mixT = P.sb("mixT", [128, 16, 512], BF16)
        W2 = 1024 if l == 0 else 2048
        ta = P.sb("ta", [128, W2]); tb = P.sb("tb", [128, W2]); tc = P.sb("tc", [128, W2])
        fa = P.sb("fa", [128, 512]); fb = P.sb("fb", [128, 512]); fc = P.sb("fc", [128, 512])
        hrow = P.sb("hrow", [128, 1024]); t1 = P.sb("t1", [128, 1024]); t2 = P.sb("t2", [128, 1024])
        ss = P.sb("ss", [128, 16]); rs = P.sb("rs", [128, 16])
        for (tok0, ntok, seg, is_ctx) in tiles:
            r = 1 if is_ctx else 0
            self.make_uT(l, hsrc, tok0, ntok, r, 0, 1, uT)
            if l == 0:
                for cc in range(8):
                    ps = pb[2 + cc % 2]
                    for k in range(8):
                        P.mm(ps[:, :ntok], Wg2[:, k, cc * 128:(cc + 1) * 128], uT[:, k, :ntok], start=(k == 0), stop=(k == 7))
                    P.act(fa[:, :ntok], ps[:, :ntok], AF.Square)
                    P.ts("dve", fa[:, :ntok], fa[:, :ntok], 0.044715, 1.0, ALU.mult, ALU.add)
                    P.tt("dve", fa[:, :ntok], fa[:, :ntok], ps[:, :ntok], ALU.mult)
                    P.act(fa[:, :ntok], fa[:, :ntok], AF.Sigmoid, scale=1.5957691216057308)
                    P.tt("dve", fa[:, :ntok], fa[:, :ntok], ps[:, :ntok], ALU.mult)
                    P.dma("sp", fb[:, :ntok], oA["rg"][cc * 128:(cc + 1) * 128, tok0:tok0 + ntok])
                    P.dma("sp", fc[:, :ntok], oB["rg"][cc * 128:(cc + 1) * 128, tok0:tok0 + ntok])
                    P.tt("dve", fb[:, :ntok], fb[:, :ntok], fc[:, :ntok], ALU.add)
                    P.tt("dve", mixT[:, cc, :ntok], fb[:, :ntok], fa[:, :ntok], ALU.mult)
            for jb in range(ntok // 128):
                bs = slice(jb * 128, (jb + 1) * 128)
                ta0 = tok0 + jb * 128
                nb = W2 // 512
                for q in range(nb):
                    c0 = (1024 if l == 0 else 0) + q * 512
                    for k in range(8):
                        P.mm(pb[2 + q], uT[:, k, bs], Wg2[:, k, c0:c0 + 512], start=(k == 0), stop=(k == 7))
                    P.act(ta[:, q * 512:(q + 1) * 512], pb[2 + q], AF.Silu)
                if l == 0:
                    P.dma("sp", tb, oA["ssd"][ta0:ta0 + 128, :])
                    P.dma("sp", tc, oB["ssd"][ta0:ta0 + 128, :])
                else:
                    P.dma("sp", tb, oA["o"][ta0:ta0 + 128, :])
                    P.dma("sp", tc, oB["o"][ta0:ta0 + 128, :])
                P.tt("dve", tb, tb, tc, ALU.add)
                if l == 0:
                    ngr, gsz = 2, 512
                    P.tt("dve", tb, tb, ta, ALU.mult)
                else:
                    ngr, gsz = 16, 128
                P.tt("dve", tc, tb, tb, ALU.mult)
                P.red("dve", ss[:, 0:ngr], tc.re("p (g c) -> p g c", c=gsz), ALU.add)
                P.ts("dve", ss[:, 0:ngr], ss[:, 0:ngr], 1.0 / gsz, None, ALU.mult)
                P.act(rs[:, 0:ngr], ss[:, 0:ngr], AF.Sqrt, bias=self.epsln[:, 1:2])
                P.recip(rs[:, 0:ngr], rs[:, 0:ngr])
                P.tt("dve", tb.re("p (g c) -> p g c", c=gsz), tb.re("p (g c) -> p g c", c=gsz),
                     rs[:, 0:ngr].un(2).bc([128, ngr, gsz]), ALU.mult)
                P.tt("dve", tb, tb, ng, ALU.mult)
                if l == 1:
                    P.tt("dve", tb, tb, ta, ALU.mult)
                cbase = 8 if l == 0 else 0
                for q in range(nb):
                    for qq in range(4):
                        P.tr(pb[4 + q % 2][:, qq * 128:(qq + 1) * 128], tb[:, (q * 4 + qq) * 128:(q * 4 + qq + 1) * 128], self.ident)
                    P.cp("act", mixT[:, cbase + q * 4:cbase + q * 4 + 4, bs], pb[4 + q % 2].re("p (a b) -> p a b", b=128))
                P.dma("sp", hrow, hsrc[ta0:ta0 + 128, :])
                for hf in range(2):
                    for c in range(16):
                        P.mm(pb[6 + hf], mixT[:, c, bs], Wo[:, c, hf * 512:(hf + 1) * 512], start=(c == 0), stop=(c == 15))
                    P.tt("dve", t1[:, hf * 512:(hf + 1) * 512], pb[6 + hf], gmix[r][:, hf * 512:(hf + 1) * 512], ALU.mult)
                P.stt("dve", t2, hrow, ALPHA_RES, t1, ALU.mult, ALU.add)
                self.ln_rows(t2, lng, lnb, t1, hrow)
                P.dma("sp", hdst[ta0:ta0 + 128, :], t1)
        P.pop()

    def phase_moe(self, l, parts, hsrc, hdst, nctx_blocks):
        P = self.P
        pb = self.pb
        for (p0, pn) in parts:
            nblk = pn // 128
            P.push()
            self.small_consts()
            vT = P.sb("vT", [128, 8, pn], BF16)
            acc = P.sb("acc", [128, nblk, 1024])
            G = P.sb("G", [128, nblk, 32])
            P.push()
            Wr = P.sb("Wr", [128, 8, 36])
            P.dma("sp", Wr, self.d["router_w"][l].re("(k p) c -> p k c", p=128))
            rb = P.sb("rb", [128, 36]); self.bcast_row("rb%d" % l, rb)
            u32 = P.sb("u32", [128, 8, 128])
            R = {n: P.sb("r_" + n, [128, w]) for n, w in [("lg", 36), ("mg", 1), ("nmg", 1), ("eg", 4), ("sg", 1), ("ptg", 1),
                                                          ("maskg", 4), ("sel3", 32), ("sel", 8), ("m1", 1), ("mask1", 8), ("sel2", 8),
                                                          ("m2", 1), ("mask2", 8), ("dd", 1), ("g1", 1), ("g2", 1), ("w8", 8)]}
            for jb in range(nblk):
                ta0 = p0 + jb * 128
                r = 1 if ta0 < NCTX else 0
                self.make_uT(l, hsrc, ta0, 128, r, 3, 4, vT[:, :, jb * 128:(jb + 1) * 128], u32=u32)
                for k in range(8):
                    P.mm(pb[2][:, 0:36], u32[:, k, :], Wr[:, k, :], start=(k == 0), stop=(k == 7))
                lg = R["lg"]
                P.tt("dve", lg, pb[2][:, 0:36], rb, ALU.add)
                P.red("dve", R["mg"], lg[:, 0:4], ALU.max)
                P.ts("dve", R["nmg"], R["mg"], -1.0, None, ALU.mult)
                P.act(R["eg"], lg[:, 0:4], AF.Exp, bias=R["nmg"], accum=R["sg"])
                P.recip(R["ptg"], R["sg"])
                P.ts("dve", R["maskg"], lg[:, 0:4], R["mg"], None, ALU.is_ge)
                P.tt("dve", R["sel3"].re("p (g e) -> p g e", e=8), lg[:, 4:36].re("p (g e) -> p g e", e=8),
                     R["maskg"].un(2).bc([128, 4, 8]), ALU.mult)
                P.red("dve", R["sel"], R["sel3"].re("p (g e) -> p e g", e=8), ALU.add)
                P.red("dve", R["m1"], R["sel"], ALU.max)
                P.ts("dve", R["mask1"], R["sel"], R["m1"], None, ALU.is_ge)
                P.stt("dve", R["sel2"], R["mask1"], -1e30, R["sel"], ALU.mult, ALU.add)
                P.red("dve", R["m2"], R["sel2"], ALU.max)
                P.ts("dve", R["mask2"], R["sel2"], R["m2"], None, ALU.is_ge)
                P.tt("dve", R["dd"], R["m1"], R["m2"], ALU.subtract)
                P.act(R["dd"], R["dd"], AF.Sigmoid)
                P.tt("dve", R["g1"], R["ptg"], R["dd"], ALU.mult)
                P.tt("dve", R["g2"], R["ptg"], R["g1"], ALU.subtract)
                P.ts("dve", R["w8"], R["mask1"], R["g1"], None, ALU.mult)
                P.stt("dve", R["w8"], R["mask2"], R["g2"], R["w8"], ALU.mult, ALU.add)
                P.tt("dve", G[:, jb, :].re("p (g e) -> p g e", e=8), R["maskg"].un(2).bc([128, 4, 8]),
                     R["w8"].un(1).bc([128, 4, 8]), ALU.mult)
            P.pop()
            P.push()
            wg = [P.sb("wg%d" % i, [128, 8, 512], BF16) for i in range(2)]
            wu = [P.sb("wu%d" % i, [128, 8, 512], BF16) for i in range(2)]
            wd = [P.sb("wd%d" % i, [128, 4, 1024], BF16) for i in range(2)]
            hT = P.sb("hT", [128, 4, 512], BF16)
            sg = P.sb("sgl", [128, 512])
            eg_, eu_, ed_ = self.d["exp_w_gate"], self.d["exp_w_up"], self.d["exp_w_down"]
            ttiles = [(a, min(512, pn - a)) for a in range(0, pn, 512)]
            for e in range(32):
                i = e % 2
                P.dma("pool", wg[i], eg_[l, e].re("(k p) c -> p k c", p=128))
                P.dma("pool", wu[i], eu_[l, e].re("(k p) c -> p k c", p=128))
                P.dma("pool", wd[i], ed_[l, e].re("(c p) d -> p c d", p=128))
                for (a, n) in ttiles:
                    for dc in range(4):
                        psg, psu = pb[dc % 2], pb[2 + dc % 2]
                        for k in range(8):
                            P.mm(psg[:, :n], wg[i][:, k, dc * 128:(dc + 1) * 128], vT[:, k, a:a + n], start=(k == 0), stop=(k == 7))
                        for k in range(8):
                            P.mm(psu[:, :n], wu[i][:, k, dc * 128:(dc + 1) * 128], vT[:, k, a:a + n], start=(k == 0), stop=(k == 7))
                        P.act(sg[:, :n], psg[:, :n], AF.Silu)
                        P.tt("dve", hT[:, dc, :n], sg[:, :n], psu[:, :n], ALU.mult)
                    for tbk in range(n // 128):
                        blk = a // 128 + tbk
                        for hf in range(2):
                            pso = pb[4 + (2 * tbk + hf) % 4]
                            for dc in range(4):
                                P.mm(pso, hT[:, dc, tbk * 128:(tbk + 1) * 128], wd[i][:, dc, hf * 512:(hf + 1) * 512],
                                     start=(dc == 0), stop=(dc == 3))
                            dst = acc[:, blk, hf * 512:(hf + 1) * 512]
                            if e == 0:
                                P.ts("dve", dst, pso, G[:, blk, e:e + 1], None, ALU.mult)
                            else:
                                P.stt("dve", dst, pso, G[:, blk, e:e + 1], dst, ALU.mult, ALU.add)
            P.pop()
            P.push()
            gffn = [P.sb("gffn%d" % r, [128, 1024]) for r in range(2)]
            for r in range(2):
                P.dma("sp", gffn[r], self.d["modrow%d" % l][r:r + 1, 1024:2048].pbc([128, 1024]))
            lng = P.sb("lnfg", [128, 1024]); self.bcast_row("lnfg%d" % l, lng)
            lnb = P.sb("lnfb", [128, 1024]); self.bcast_row("lnfb%d" % l, lnb)
            hrow = P.sb("hrow", [128, 1024]); t1 = P.sb("t1", [128, 1024]); t2 = P.sb("t2", [128, 1024])
            for jb in range(nblk):
                ta0 = p0 + jb * 128
                r = 1 if ta0 < NCTX else 0
                P.dma("sp", hrow, hsrc[ta0:ta0 + 128, :])
                P.tt("dve", t1, acc[:, jb, :], gffn[r], ALU.mult)
                P.stt("dve", t2, hrow, ALPHA_RES, t1, ALU.mult, ALU.add)
                self.ln_rows(t2, lng, lnb, t1, hrow)
                P.dma("sp", hdst[ta0 - (0 if nctx_blocks else NCTX):ta0 - (0 if nctx_blocks else NCTX) + 128, :], t1)
            P.pop()
            P.pop()
POOL = "dve"
class KB3(KB2):
    def gdn_begin(self, d):
        P = self.P
        P.push()
        W = self.d["cd_w"]
        self.Wqkv = P.sb("Wqkv", [128, 8, 3072], BF16)
        self.load_w(self.Wqkv, W, 0, 3072)
        self.Wba = P.sb("Wba", [128, 8, 16], BF16)
        self.load_w(self.Wba[:, :, 0:8], W, 4096 + 8 * d, 8)
        self.load_w(self.Wba[:, :, 8:16], W, 4112 + 8 * d, 8)
        self.negAg = P.sb("negAg", [128, 16])
        self.bcast_row("galog", self.negAg)
        P.act(self.negAg, self.negAg, AF.Exp)
        P.ts("dve", self.negAg, self.negAg, -1.0, None, ALU.mult)
        self.gdtb = P.sb("gdtb", [128, 16])
        self.bcast_row("gdtb", self.gdtb)
        self.small_consts()
        self.S = P.sb("gS", [128, 1024])
        self.Sbf = P.sb("gSbf", [128, 1024], BF16)
        self.uT = P.sb("uT", [128, 8, 256], BF16)
        self.qTb = P.sb("qTb", [128, 8, 256], BF16)
        self.kTb = P.sb("kTb", [128, 8, 256], BF16)
        self.kT32 = P.sb("kT32", [128, 8, 256])
        self.vT32 = P.sb("vT32", [128, 8, 256])
        self.fxs = [P.sb("fx%d" % i, [128, 256]) for i in range(3)]
        self.q32 = P.sb("q32", [128, 8, 256])
        self.sqr = P.sb("sqr", [128, 8, 256])
        self.g_gt = P.sb("g_gt", [128, 64])
        self.g_raw = P.sb("g_raw", [128, 16])
        self.g_V = P.sb("g_V", [128, 8, 128]); self.g_dg = P.sb("g_dg", [128, 8, 128])
        self.g_Lb = P.sb("g_Lb", [128, 8, 128]); self.g_L = P.sb("g_L", [128, 8, 128])
        self.g_A = P.sb("g_A", [128, 8, 128])
        self.g_attn = P.sb("g_attn", [128, 8, 128], BF16)

        def halves(name, dtype=F32):
            return [P.sb("%s%d" % (name, q), [128, 4, 128], dtype) for q in range(2)]
        self.g_Pm = halves("g_Pm"); self.g_PT = halves("g_PT"); self.g_IPT = halves("g_IPT"); self.g_Y = halves("g_Y")
        self.g_ktok = P.sb("g_ktok", [128, 8, 128])
        self.g_vb = P.sb("g_vb", [128, 8, 128])
        self.g_kbe = P.sb("g_kbe", [128, 8, 128])
        self.g_kw = P.sb("g_kw", [128, 8, 128], BF16)
        self.g_nwT = halves("g_nwT")
        self.g_vnb = halves("g_vnb", BF16)
        self.g_o = P.sb("g_o", [128, 1024]); self.g_tmp = P.sb("g_tmp", [128, 1024])

    def gdn_state_init(self, st_in):
        P = self.P
        if st_in is None:
            P.memset("dve", self.S, 0.0)
        else:
            P.dma("sp", self.S, st_in[0])
        P.cp("act", self.Sbf, self.S)

    def gdn_state_out(self, st_out):
        self.P.dma("sp", st_out[0], self.S)

    def gdn_tile(self, d, tile, o_dst, hsrc):
        P = self.P
        pb = self.pb
        tok0, ntok, seg, is_ctx = tile
        r = 1 if is_ctx else 0
        uT = self.uT
        self.make_uT(1, hsrc, tok0, ntok, r, 0, 1, uT)
        for cc in range(24):
            ps = pb[2 + cc % 6]
            fx = self.fxs[cc % 3]
            for k in range(8):
                P.mm(ps[:, :ntok], self.Wqkv[:, k, cc * 128:(cc + 1) * 128], uT[:, k, :ntok], start=(k == 0), stop=(k == 7))
            self.conv(fx, ps, self.pcs("gdnconv", cc * 5, 5), None, ntok, seg)
            h = cc % 8
            dst = self.q32 if cc < 8 else (self.kT32 if cc < 16 else self.vT32)
            P.act(dst[:, h, :ntok], fx[:, :ntok], AF.Silu)
        for which in range(2):
            src = self.q32 if which == 0 else self.kT32
            sq = self.sqr
            P.tt("dve", sq[:, :, :ntok], src[:, :, :ntok], src[:, :, :ntok], ALU.mult)
            for h in range(8):
                P.mm(pb[2 + h // 2][:, (h % 2) * 256:(h % 2) * 256 + ntok], self.ones, sq[:, h, :ntok])
            for b in range(4):
                o3 = sq[:, 2 * b:2 * b + 2, :ntok]
                i3 = pb[2 + b].re("p (a c) -> p a c", c=256)[:, :, :ntok]
                P.act(o3, i3, AF.Ln, bias=self.epsln[:, 1:2])
            for b in range(4):
                o3 = sq[:, 2 * b:2 * b + 2, :ntok]
                P.act(o3, o3, AF.Exp, scale=-0.5)
            if which == 0:
                P.stt("dve", self.qTb[:, :, :ntok], src[:, :, :ntok], 128.0 ** -0.5, sq[:, :, :ntok], ALU.mult, ALU.mult)
            else:
                P.tt("dve", self.kT32[:, :, :ntok], src[:, :, :ntok], sq[:, :, :ntok], ALU.mult)
                P.cp("act", self.kTb[:, :, :ntok], self.kT32[:, :, :ntok])
        nch = ntok // 128
        order = range(nch - 1, -1, -1) if d == 1 else range(nch)
        for ci in order:
            self.gdn_chunk(d, ci * 128, tok0 + ci * 128, o_dst)

    def gdn_chunk(self, d, c0, tok_abs, o_dst):
        P = self.P
        pb, slots = self.pb, self.slots
        dirB = d == 1
        cs = slice(c0, c0 + 128)
        M1 = self.MLT if dirB else self.MGT
        M2 = self.MGE if dirB else self.MLE
        Mstrict = self.MGT if dirB else self.MLT
        Mincl = M2
        uT = self.uT
        gt = self.g_gt
        bt, lb, g, tmp8 = gt[:, 0:8], gt[:, 8:16], gt[:, 16:24], gt[:, 24:32]
        E = gt[:, 32:56]
        bec = gt[:, 56:64]
        for k in range(8):
            P.mm(pb[6][:, 0:16], uT[:, k, cs], self.Wba[:, k, :], start=(k == 0), stop=(k == 7))
        P.cp("dve", self.g_raw, pb[6][:, 0:16])
        P.act(bt, self.g_raw[:, 0:8], AF.Sigmoid)
        P.act(lb, bt, AF.Ln)
        P.tt("dve", tmp8, self.g_raw[:, 8:16], self.gdtb[:, d * 8:(d + 1) * 8], ALU.add)
        P.act(tmp8, tmp8, AF.Exp)
        P.act(tmp8, tmp8, AF.Ln, bias=1.0)
        P.tt("dve", g, tmp8, self.negAg[:, d * 8:(d + 1) * 8], ALU.mult)
        P.mm(pb[7][:, 0:8], M2, g)
        P.mm(pb[7][:, 8:16], M1, g)
        P.mm(pb[7][:, 16:24], self.ones, g)
        P.act(E, pb[7][:, 0:24], AF.Exp)
        ecum, wend, etot = E[:, 0:8], E[:, 8:16], E[:, 16:24]
        P.tt("dve", bec, bt, ecum, ALU.mult)
        Vt, dg = self.g_V, self.g_dg
        P.tt("dve", Vt, M1.un(1).bc([128, 8, 128]), g.un(2).bc([128, 8, 128]), ALU.mult)
        P.tt("dve", dg, self.ident.un(1).bc([128, 8, 128]), lb.un(2).bc([128, 8, 128]), ALU.mult)
        for h in range(8):
            s1 = slots[h // 4][h % 4]
            P.mm(s1, Vt[:, h, :], M2, start=True, stop=False)
            P.mm(s1, self.ones, dg[:, h, :], start=False, stop=True)
            P.mm(slots[2 + h // 4][h % 4], Vt[:, h, :], M2)
        Lb, L = self.g_Lb, self.g_L
        for q in range(2):
            P.act(Lb[:, q * 4:(q + 1) * 4, :].re("p a b -> p (a b)"), pb[q], AF.Exp)
            P.act(L[:, q * 4:(q + 1) * 4, :].re("p a b -> p (a b)"), pb[2 + q], AF.Exp)
        for h in range(8):
            P.mm(slots[4 + h // 4][h % 4], self.kTb[:, h, cs], self.kTb[:, h, cs])
            P.mm(slots[6 + h // 4][h % 4], self.kTb[:, h, cs], self.qTb[:, h, cs])
        A = self.g_A
        for q in range(2):
            P.tt("dve", A[:, q * 4:(q + 1) * 4, :], pb[4 + q].re("p (a b) -> p a b", b=128), Lb[:, q * 4:(q + 1) * 4, :], ALU.mult)
            P.tt("dve", L[:, q * 4:(q + 1) * 4, :], pb[6 + q].re("p (a b) -> p a b", b=128), L[:, q * 4:(q + 1) * 4, :], ALU.mult)
        P.tt("dve", A, A, Mstrict.un(1).bc([128, 8, 128]), ALU.mult)
        P.tt(POOL, self.g_attn, L, Mincl.un(1).bc([128, 8, 128]), ALU.mult)
        Pm, PT, IPT, Y = self.g_Pm, self.g_PT, self.g_IPT, self.g_Y
        i3 = self.ident.un(1).bc([128, 4, 128])

        def b3(i):
            return pb[i].re("p (a b) -> p a b", b=128)
        for q in range(2):
            for hh in range(4):
                P.tr(slots[q][hh], A[:, q * 4 + hh, :], self.ident)
        for q in range(2):
            P.cp("act", PT[q], b3(q))
            P.tt("dve", Y[q], i3, A[:, q * 4:(q + 1) * 4, :], ALU.subtract)
            P.cp(POOL, Pm[q], A[:, q * 4:(q + 1) * 4, :])
        NL = 6
        for lev in range(NL):
            last = lev == NL - 1
            for q in range(2):
                if not last:
                    for hh in range(4):
                        P.mm(slots[q][hh], PT[q][:, hh, :], Pm[q][:, hh, :])
                for hh in range(4):
                    P.mm(slots[2 + q][hh], Pm[q][:, hh, :], PT[q][:, hh, :])
            for q in range(2):
                if not last:
                    P.cp("act", PT[q], b3(2 + q))
                    P.tt("dve", IPT[q], PT[q], i3, ALU.add)
                    P.cp("act", Pm[q], b3(q))
                else:
                    P.tt("dve", IPT[q], b3(2 + q), i3, ALU.add)
                for hh in range(4):
                    P.mm(slots[4 + q][hh], IPT[q][:, hh, :], Y[q][:, hh, :])
                P.cp("dve", Y[q], b3(4 + q))
        for h in range(8):
            P.tr(slots[6 + h // 4][h % 4], self.kT32[:, h, cs], self.ident)
        for q in range(2):
            P.cp("act", self.g_ktok[:, q * 4:(q + 1) * 4, :], pb[6 + q].re("p (a b) -> p a b", b=128))
        for h in range(8):
            P.tr(slots[6 + h // 4][h % 4], self.vT32[:, h, cs], self.ident)
        for q in range(2):
            P.tt("dve", self.g_vb[:, q * 4:(q + 1) * 4, :], pb[6 + q].re("p (a b) -> p a b", b=128),
                 bt[:, q * 4:(q + 1) * 4].un(2).bc([128, 4, 128]), ALU.mult)
        P.tt("dve", self.g_kbe, self.g_ktok, bec.un(2).bc([128, 8, 128]), ALU.mult)
        P.tt(POOL, self.g_kw, self.g_ktok, wend.un(2).bc([128, 8, 128]), ALU.mult)
        S3 = self.S.re("p (h v) -> p h v", v=128)
        Sb3 = self.Sbf.re("p (h v) -> p h v", v=128)
        nwT, vnb = self.g_nwT, self.g_vnb
        for q in range(2):
            for hh in range(4):
                P.mm(slots[q][hh], self.g_kbe[:, q * 4 + hh, :], Y[q][:, hh, :])
            P.ts("dve", nwT[q], b3(q), -1.0, None, ALU.mult)
        for q in range(2):
            for hh in range(4):
                h = q * 4 + hh
                P.mm(slots[2 + q][hh], Y[q][:, hh, :], self.g_vb[:, h, :], start=True, stop=False)
                P.mm(slots[2 + q][hh], nwT[q][:, hh, :], S3[:, h, :], start=False, stop=True)
            P.cp("act", vnb[q], b3(2 + q))
        for q in range(2):
            for hh in range(4):
                h = q * 4 + hh
                P.mm(slots[4 + q][hh], self.qTb[:, h, cs], Sb3[:, h, :])
                P.mm(slots[6 + q][hh], self.g_attn[:, h, :], vnb[q][:, hh, :])
                P.mm(slots[q][hh], self.g_kw[:, h, :], vnb[q][:, hh, :])
        o, tmp = self.g_o, self.g_tmp
        for q in range(2):
            P.cp("act", o[:, q * 512:(q + 1) * 512], pb[6 + q])
            P.tt("dve", tmp[:, q * 512:(q + 1) * 512].re("p (a b) -> p a b", b=128), pb[4 + q].re("p (a b) -> p a b", b=128),
                 ecum[:, q * 4:(q + 1) * 4].un(2).bc([128, 4, 128]), ALU.mult)
        P.tt("dve", o, o, tmp, ALU.add)
        if o_dst is not None:
            P.dma("sp", o_dst[tok_abs:tok_abs + 128, 0:1024], o)
        for h in range(8):
            P.stt("dve", S3[:, h, :], S3[:, h, :], etot[:, h:h + 1], slots[h // 4][h % 4], ALU.mult, ALU.add)
        P.cp("act", self.Sbf, self.S)

    def hgrn_begin(self, d):
        P = self.P
        P.push()
        W = self.d["cd_w"]
        self.Whq = P.sb("Whq", [128, 8, 1024], BF16); self.load_w(self.Whq, W, 4128, 1024)
        self.Whf = P.sb("Whf", [128, 8, 1024], BF16); self.load_w(self.Whf, W, 5152 + 1024 * d, 1024)
        self.Whi = P.sb("Whi", [128, 8, 1024], BF16); self.load_w(self.Whi, W, 7200, 1024)
        self.hlb = P.sb("hlb", [128, 8]); self.homl = P.sb("homl", [128, 8])
        P.tt("dve", self.hlb, self.pcs("hglb1"), self.pcs("hglb0"), ALU.subtract)
        P.act(self.hlb, self.hlb, AF.Sigmoid)
        P.ts("dve", self.homl, self.hlb, -1.0, 1.0, ALU.mult, ALU.add)
        self.S = P.sb("hS", [128, 1024])
        self.Sbf = P.sb("hSbf", [128, 1024], BF16)
        self.uT = P.sb("uT", [128, 8, 512], BF16)
        self.qtb = P.sb("qtb", [128, 8, 512], BF16)
        self.ktb = P.sb("ktb", [128, 8, 512], BF16)
        self.kwtok = P.sb("kwtok", [64, 8, 8, 128], BF16)
        self.ect = P.sb("ect", [128, 8, 8])
        nm = ["f", "lf", "kk", "cum", "E", "t1"]
        self.hts = [{n: P.sb("h_%s%d" % (n, i), [128, 512]) for n in nm} for i in range(2)]
        self.h_vtok = P.sb("h_vtok", [64, 1024], BF16)
        self.h_attn = P.sb("h_attn", [64, 8, 64], BF16)
        self.h_o = P.sb("h_o", [64, 1024])

    def hgrn_state_init(self, st_in):
        P = self.P
        if st_in is None:
            P.memset("dve", self.S, 0.0)
        else:
            P.dma("sp", self.S, st_in[1])
        P.cp("act", self.Sbf, self.S)

    def hgrn_state_out(self, st_out):
        self.P.dma("sp", st_out[1], self.S)

    def hgrn_tile(self, d, tile, o_dst, hsrc):
        P = self.P
        pb, slots = self.pb, self.slots
        tok0, ntok, seg, is_ctx = tile
        dirB = d == 1
        r = 1 if is_ctx else 0
        uT = self.uT
        self.make_uT(1, hsrc, tok0, ntok, r, 0, 1, uT)
        nc_ = ntok // 64
        n = ntok
        for h in range(8):
            T = self.hts[h % 2]
            f, lf, kk, cum, E, t1 = T["f"], T["lf"], T["kk"], T["cum"], T["E"], T["t1"]
            psq, psf = (pb[2], pb[3]) if h % 2 == 0 else (pb[6], pb[7])
            for k in range(8):
                P.mm(psq[:, :n], self.Whq[:, k, h * 128:(h + 1) * 128], uT[:, k, :n], start=(k == 0), stop=(k == 7))
            for k in range(8):
                P.mm(psf[:, :n], self.Whf[:, k, h * 128:(h + 1) * 128], uT[:, k, :n], start=(k == 0), stop=(k == 7))
            P.act(f[:, :n], psf[:, :n], AF.Sigmoid)
            P.ts("dve", f[:, :n], f[:, :n], self.homl[:, h:h + 1], self.hlb[:, h:h + 1], ALU.mult, ALU.add)
            P.act(lf[:, :n], f[:, :n], AF.Ln)
            P.ts("dve", kk[:, :n], f[:, :n], -1.0, 1.0, ALU.mult, ALU.add)
            if not dirB:
                P.scan(cum[:, :n], self.RESET[:, 0:n], lf[:, :n], 0.0)
            else:
                P.scan(cum[:, :n][:, ::-1], self.RESET[:, 1:n + 1][:, ::-1], lf[:, :n][:, ::-1], 0.0)
            P.act(E[:, :n], cum[:, :n], AF.Exp)
            P.tt("dve", self.qtb[:, h, :n], psq[:, :n], E[:, :n], ALU.mult)
            c3 = cum[:, :n].re("p (c s) -> p c s", s=64)
            E3 = E[:, :n].re("p (c s) -> p c s", s=64)
            pos = 0 if dirB else 63
            P.cp("dve", self.ect[:, h, 0:nc_], E3[:, :, pos])
            P.act(E[:, :n], cum[:, :n], AF.Exp, scale=-1.0)
            P.tt("dve", self.ktb[:, h, :n], kk[:, :n], E[:, :n], ALU.mult)
            t3 = t1[:, :n].re("p (c s) -> p c s", s=64)
            P.tt("dve", t3, c3[:, :, pos:pos + 1].bc([128, nc_, 64]), c3, ALU.subtract)
            P.act(t1[:, :n], t1[:, :n], AF.Exp)
            P.tt("dve", t1[:, :n], t1[:, :n], kk[:, :n], ALU.mult)
            for c in range(nc_):
                P.tr(pb[4 + (c // 4) % 2][0:64, (c % 4) * 128:(c % 4 + 1) * 128], t1[:, c * 64:(c + 1) * 64], self.ident)
            for q in range((nc_ + 3) // 4):
                m = min(4, nc_ - q * 4)
                P.cp("act", self.kwtok[:, q * 4:q * 4 + m, h, :], pb[4 + q % 2][0:64, 0:m * 128].re("p (a b) -> p a b", b=128))
        order = range(nc_ - 1, -1, -1) if dirB else range(nc_)
        Mv = (self.MGE if dirB else self.MLE)[0:64, 0:64]
        S3 = self.S.re("p (h v) -> p h v", v=128)
        Sb3 = self.Sbf.re("p (h v) -> p h v", v=128)
        for c in order:
            cc = slice(c * 64, (c + 1) * 64)
            for hf in range(2):
                for k in range(8):
                    P.mm(pb[6 + hf][0:64, :], uT[:, k, cc], self.Whi[:, k, hf * 512:(hf + 1) * 512], start=(k == 0), stop=(k == 7))
                P.cp("act", self.h_vtok[:, hf * 512:(hf + 1) * 512], pb[6 + hf][0:64, :])
            for h in range(8):
                P.mm(pb[2][0:64, h * 64:(h + 1) * 64], self.ktb[:, h, cc], self.qtb[:, h, cc])
            P.tt("dve", self.h_attn, pb[2][0:64, :].re("p (h t) -> p h t", t=64), Mv.un(1).bc([64, 8, 64]), ALU.mult)
            for h in range(8):
                so = pb[4 + h // 4][0:64, (h % 4) * 128:(h % 4 + 1) * 128]
                P.mm(so, self.h_attn[:, h, :], self.h_vtok[:, h * 128:(h + 1) * 128], start=True, stop=False)
                P.mm(so, self.qtb[:, h, cc], Sb3[:, h, :], start=False, stop=True)
            for h in range(8):
                P.mm(slots[h // 4][h % 4], self.kwtok[:, c, h, :], self.h_vtok[:, h * 128:(h + 1) * 128])
            for q in range(2):
                P.cp("act", self.h_o[:, q * 512:(q + 1) * 512], pb[4 + q][0:64, :])
            if o_dst is not None:
                ta = tok0 + c * 64
                P.dma("sp", o_dst[ta:ta + 64, 1024:2048], self.h_o)
            P.tt("dve", S3, S3, self.ect[:, :, c:c + 1].bc([128, 8, 128]), ALU.mult)
            for q in range(2):
                P.tt("dve", self.S[:, q * 512:(q + 1) * 512], self.S[:, q * 512:(q + 1) * 512], pb[q], ALU.add)
            P.cp("act", self.Sbf, self.S)


def tiles_256():
    t = [(0, 256, 256, True)]
    for i in range(16):
        t.append((NCTX + i * 256, 256, 64, False))
    return t
def _common_ins(kb, with_ab, with_cd, with_moe):
    kb.ext_in("consts", [128, NCONST]); kb.ext_in("pcol", [128, PC.n]); kb.ext_in("pvec", [1, PV.n])
    kb.ext_in("ada_w", [2, D, 6 * D])
    if with_ab:
        kb.ext_in("ab_w", [D, AB_IN]); kb.ext_in("rg_gw", [2, 2, 8, 128, 128])
    if with_cd:
        kb.ext_in("cd_w", [D, CD_IN])
    if with_moe:
        kb.ext_in("mix_out_w", [2, 2048, D]); kb.ext_in("router_w", [2, D, 36])
        kb.ext_in("exp_w_gate", [2, 32, D, 512]); kb.ext_in("exp_w_up", [2, 32, D, 512]); kb.ext_in("exp_w_down", [2, 32, 512, D])


def emit_l0_passA(kb, hsrc, oA_rg, oA_ssd, stA):
    kb.mixer_ab_begin(0)
    kb.mixer_state_init(None)
    for t in tiles_all():
        kb.mixer_ab_tile(0, t, oA_rg, oA_ssd, True, hsrc)
    kb.mixer_state_out(stA)
    kb.mixer_end()


def emit_l0_passB(kb, hsrc, oB_rg, oB_ssd, stB):
    kb.mixer_ab_begin(1)
    kb.mixer_state_init(None)
    tl = tiles_all()
    kb.mixer_ab_tile(1, tl[0], oB_rg, oB_ssd, False, hsrc)
    kb.mixer_state_init(stB)
    for t in reversed(tl[1:]):
        kb.mixer_ab_tile(1, t, oB_rg, oB_ssd, False, hsrc)
    kb.mixer_end()


def emit_l1_passA(kb, hsrc, oA, stA):
    kb.gdn_begin(0); kb.gdn_state_init(None)
    for t in tiles_256():
        kb.gdn_tile(0, t, None if t[3] else oA, hsrc)
    kb.gdn_state_out(stA); kb.mixer_end()
    kb.hgrn_begin(0); kb.hgrn_state_init(None)
    for t in tiles_all():
        kb.hgrn_tile(0, t, None if t[3] else oA, hsrc)
    kb.hgrn_state_out(stA); kb.mixer_end()


def emit_l1_passB(kb, hsrc, oB, stB):
    kb.gdn_begin(1); kb.gdn_state_init(stB)
    for t in reversed(tiles_256()[1:]):
        kb.gdn_tile(1, t, oB, hsrc)
    kb.mixer_end()
    kb.hgrn_begin(1); kb.hgrn_state_init(stB)
    for t in reversed(tiles_all()[1:]):
        kb.hgrn_tile(1, t, oB, hsrc)
    kb.mixer_end()


def emit_exchange(kb, st_local, gath, st_partner):
    P = kb.P
    for c in range(2):
        P.collective("AllGather", gath[c], st_local[c], [list(range(8))])
    P.push()
    for c in range(2):
        g = P.sb("xg%d" % c, [128, 8, 1024])
        acc = P.sb("xacc%d" % c, [128, 1024])
        P.dma("sp", g, gath[c].re("(r p) w -> p r w", p=128))
        for r in range(8):
            sel = kb.pcs("psel", r)
            if r == 0:
                P.ts("dve", acc, g[:, r, :], sel, None, ALU.mult)
            else:
                P.stt("dve", acc, g[:, r, :], sel, acc, ALU.mult, ALU.add)
        P.dma("sp", st_partner[c], acc)
    P.pop()


def build_fused():
    kb = KB3()
    _common_ins(kb, True, True, True)
    kb.ext_in("h0", [NT, D])
    for l in range(2):
        kb.scratch("modrow%d" % l, [2, 2048])
    kb.scratch("oA_rg", [D, NT]); kb.scratch("oA_ssd", [NT, D]); kb.scratch("oB_rg", [D, NT]); kb.scratch("oB_ssd", [NT, D])
    for l in range(2):
        for c in range(2):
            kb.scratch("stA%d_%d" % (l, c), [128, 1024]); kb.scratch("gath%d_%d" % (l, c), [1024, 1024])
            kb.scratch("stB%d_%d" % (l, c), [128, 1024])
    kb.scratch("h1", [NT, D]); kb.scratch("h2", [NT, D]); kb.scratch("h3", [NT, D])
    kb.scratch("oA1", [NT, 2048]); kb.scratch("oB1", [NT, 2048])
    kb.ext_out("out", [NLAT, D])
    kb.setup()
    d = kb.d
    stA = [[d["stA%d_%d" % (l, c)] for c in range(2)] for l in range(2)]
    stB = [[d["stB%d_%d" % (l, c)] for c in range(2)] for l in range(2)]
    gath = [[d["gath%d_%d" % (l, c)] for c in range(2)] for l in range(2)]
    kb.phase_mod(0)
    emit_l0_passA(kb, d["h0"], d["oA_rg"], d["oA_ssd"], stA[0])
    emit_exchange(kb, stA[0], gath[0], stB[0])
    emit_l0_passB(kb, d["h0"], d["oB_rg"], d["oB_ssd"], stB[0])
    kb.phase_out(0, tiles_all(), d["h0"], d["h1"], {"rg": d["oA_rg"], "ssd": d["oA_ssd"]}, {"rg": d["oB_rg"], "ssd": d["oB_ssd"]})
    kb.phase_moe(0, [(0, 2176), (2176, 2176)], d["h1"], d["h2"], True)
    kb.phase_mod(1)
    emit_l1_passA(kb, d["h2"], d["oA1"], stA[1])
    emit_exchange(kb, stA[1], gath[1], stB[1])
    emit_l1_passB(kb, d["h2"], d["oB1"], stB[1])
    kb.phase_out(1, tiles_all()[1:], d["h2"], d["h3"], {"o": d["oA1"]}, {"o": d["oB1"]})
    kb.phase_moe(1, [(256, 2048), (2304, 2048)], d["h3"], d["out"], False)
    kb.P.finish()
    return kb


def kernel(**inputs):
    inp = {k: np.asarray(v) for k, v in inputs.items()}
    sh = prep_shared(inp)
    B = inp["x"].shape[0]
    cores = [(b, half) for b in range(B) for half in range(2)]
    n = len(cores)
    pcs = [prep_core(inp, b, half, i) for i, (b, half) in enumerate(cores)]
    kb = build_fused()
    ms = []
    for i in range(n):
        par = cores[i][1]
        ms.append({"consts": sh["consts"], "pcol": pcs[i]["pcol"], "pvec": pcs[i]["pvec"], "ada_w": inp["ada_w"],
                   "ab_w": sh["ab_w"][par], "rg_gw": sh["rg_gw"][par], "cd_w": sh["cd_w"][par],
                   "mix_out_w": inp["mix_out_w"], "router_w": sh["router_w"], "exp_w_gate": inp["exp_w_gate"],
                   "exp_w_up": inp["exp_w_up"], "exp_w_down": inp["exp_w_down"], "h0": pcs[i]["h0"]})
    res = run_bass_kernel_spmd(kb.nc, ms, core_ids=list(range(n))).results
    out = np.zeros((B, 2 * NLAT, D), np.float32)
    for i, (b, half) in enumerate(cores):
        o = np.asarray(res[i]["out"], np.float32)
        if half == 0:
            out[b, :NLAT] = o
        else:
            out[b, NLAT:] = o[::-1]
    return out
```
